# Optimizing a Trainium2 kernel written in Bass

```python
import math
import jax, jax.numpy as jnp
from jax import lax
import numpy as np

D_MODEL = 2048
BATCH = 4
SEQ = 4096
DEPTH = 4

HEAD_DIM = 128
N_MIX_HEADS = D_MODEL // HEAD_DIM
A_HEADS = N_MIX_HEADS // 4
B_HEADS = (N_MIX_HEADS - A_HEADS) // 2
C_HEADS = N_MIX_HEADS - A_HEADS - B_HEADS
A_DK = HEAD_DIM
A_DV = HEAD_DIM
A_W = A_HEADS * A_DK
B_W = B_HEADS * HEAD_DIM
C_W = C_HEADS * HEAD_DIM
MIX_W = A_W + B_W + C_W
IN_SPLITS = [A_W, A_W, A_W, A_W, B_W, B_W, B_W, B_HEADS, C_W, C_W, C_W]
IN_COLS = sum(IN_SPLITS)
A_CHUNK = 64
B_Q_BLOCK = 128
C_BLOCK = 256
C_TOPK = 3
C_Q_BLOCK = 64
D_FF = 5632
CONV_W = 3
PLE_DIM = 256
EPS = 1e-6
TINY = 1e-30
NEG = -1e30

kernel_name = "hymba_style_hgrn2_fox_moba_hybrid"


def rmsnorm(x, g):
    xf = x.astype(jnp.float32)
    y = xf * lax.rsqrt(jnp.mean(xf * xf, axis=-1, keepdims=True) + EPS)
    return (y * g.astype(jnp.float32)).astype(x.dtype)


def heads(t, n):
    b, s, _ = t.shape
    return t.reshape(b, s, n, -1).transpose(0, 2, 1, 3)


def merge_blocks(o):
    nqb, b, h, q, d = o.shape
    return o.transpose(1, 0, 3, 2, 4).reshape(b, nqb * q, h * d)


def alibi_slopes(n):
    return jnp.exp2(-8.0 * jnp.arange(1, n + 1, dtype=jnp.float32) / n)


def hgrn2_lower_bounds(lb_logits):
    sm = jax.nn.softmax(lb_logits.astype(jnp.float32), axis=0)
    return jnp.cumsum(sm, axis=0) - sm[0:1]


def hgrn2_mixer(q, f_logit, i, g, lb, o_norm_g):
    bsz, s, _ = q.shape
    dt = q.dtype
    z = f_logit.astype(jnp.float32)
    lbf = lb.astype(jnp.float32)
    f = lbf + (1.0 - lbf) * jax.nn.sigmoid(z)
    log_f = jnp.log(jnp.maximum(f, TINY))
    k = (1.0 - lbf) * jax.nn.sigmoid(-z)
    qh = heads(jax.nn.silu(q.astype(jnp.float32)), A_HEADS)
    kh = heads(k, A_HEADS)
    gh = heads(log_f, A_HEADS)
    vh = heads(i.astype(jnp.float32), A_HEADS)
    nc = s // A_CHUNK

    def chunks(t):
        return t.reshape(bsz, A_HEADS, nc, A_CHUNK, t.shape[-1]).transpose(2, 0, 1, 3, 4)

    causal = jnp.tril(jnp.ones((A_CHUNK, A_CHUNK), dtype=bool))[:, :, None]

    def step(state, xs):
        qc, kc, gc, vc = xs
        b = jnp.cumsum(gc, axis=2)
        o_inter = jnp.einsum('bhtd,bhde->bhte', qc * jnp.exp(b), state)
        diff = b[:, :, :, None, :] - b[:, :, None, :, :]
        decay = jnp.where(causal, jnp.exp(jnp.where(causal, diff, 0.0)), 0.0)
        attn = jnp.einsum('bhtd,bhsd,bhtsd->bhts', qc, kc, decay)
        o = o_inter + jnp.einsum('bhts,bhse->bhte', attn, vc)
        b_last = b[:, :, -1:, :]
        k_dec = kc * jnp.exp(b_last - b)
        state = jnp.exp(b_last[:, :, 0, :])[..., None] * state + jnp.einsum('bhsd,bhse->bhde', k_dec, vc)
        return state, o

    s0 = jnp.zeros((bsz, A_HEADS, A_DK, A_DV), jnp.float32)
    _, o = lax.scan(step, s0, (chunks(qh), chunks(kh), chunks(gh), chunks(vh)))
    o = o.transpose(1, 2, 0, 3, 4).reshape(bsz, A_HEADS, s, A_DV)
    o = rmsnorm(o, o_norm_g) * jax.nn.silu(heads(g.astype(jnp.float32), A_HEADS))
    return o.transpose(0, 2, 1, 3).reshape(bsz, s, A_W).astype(dt)


def fox_mixer(q, k, v, f_logit, b_f, qn_g, kn_g):
    bsz, s, _ = q.shape
    dt = q.dtype
    qh = rmsnorm(heads(q, B_HEADS), qn_g)
    kh = rmsnorm(heads(k, B_HEADS), kn_g)
    vh = heads(v, B_HEADS)
    log_f = jax.nn.log_sigmoid(f_logit.astype(jnp.float32) + b_f.astype(jnp.float32))
    fcum = jnp.cumsum(log_f, axis=1).transpose(0, 2, 1)
    nqb = s // B_Q_BLOCK
    qb = qh.reshape(bsz, B_HEADS, nqb, B_Q_BLOCK, HEAD_DIM).transpose(2, 0, 1, 3, 4)
    fq = fcum.reshape(bsz, B_HEADS, nqb, B_Q_BLOCK).transpose(2, 0, 1, 3)
    kpos = jnp.arange(s)
    scale = HEAD_DIM ** -0.5

    def block(xs):
        qi, fi, bi = xs
        qpos = bi * B_Q_BLOCK + jnp.arange(B_Q_BLOCK)
        logits = (jnp.einsum('bhqd,bhkd->bhqk', qi, kh).astype(jnp.float32) * scale
                  + fi[..., None] - fcum[:, :, None, :])
        logits = jnp.where(kpos[None, :] <= qpos[:, None], logits, NEG)
        w = jax.nn.softmax(logits, axis=-1).astype(dt)
        return jnp.einsum('bhqk,bhkd->bhqd', w, vh)

    o = lax.map(block, (qb, fq, jnp.arange(nqb)))
    return merge_blocks(o)


def moba_mixer(q, k, v, qn_g, kn_g, slopes):
    bsz, s, _ = q.shape
    dt = q.dtype
    h = C_HEADS
    qh = rmsnorm(heads(q, h), qn_g)
    kh = rmsnorm(heads(k, h), kn_g)
    vh = heads(v, h)
    nb = -(-s // C_BLOCK)
    s_pad = nb * C_BLOCK
    pad = ((0, 0), (0, 0), (0, s_pad - s), (0, 0))
    kp = jnp.pad(kh, pad)
    vp = jnp.pad(vh, pad)
    kblk = kp.reshape(bsz, h, nb, C_BLOCK, HEAD_DIM)
    vblk = vp.reshape(bsz, h, nb, C_BLOCK, HEAD_DIM)
    pos = jnp.arange(s)
    own = pos // C_BLOCK
    topk = min(C_TOPK, nb - 1)
    scale = HEAD_DIM ** -0.5
    kflat = kblk.reshape(bsz * h * nb, C_BLOCK, HEAD_DIM)
    vflat = vblk.reshape(bsz * h * nb, C_BLOCK, HEAD_DIM)
    bh_off = (jnp.arange(bsz * h, dtype=jnp.int32) * nb).reshape(bsz, h, 1, 1)
    blk_off = jnp.arange(C_BLOCK)
    sl = slopes[None, :, None, None]
    nqb = s // C_Q_BLOCK

    def to_blocks(t):
        t = t.reshape((bsz, h, nqb, C_Q_BLOCK) + t.shape[3:])
        return jnp.moveaxis(t, 2, 0)

    if topk > 0:
        kbar = jnp.mean(kblk.astype(jnp.float32), axis=3)
        gate = jnp.einsum('bhsd,bhnd->bhsn', qh.astype(jnp.float32), kbar)
        past = jnp.arange(nb)[None, :] < own[:, None]
        gate = jnp.where(past, gate, NEG)
        gval, gidx = lax.top_k(gate, topk)
        gvalid = gval > NEG / 2
        xs = (to_blocks(qh), jnp.arange(nqb), to_blocks(gidx), to_blocks(gvalid))
    else:
        xs = (to_blocks(qh), jnp.arange(nqb))

    def block(xs):
        qi, bi = xs[0], xs[1]
        qpos = bi * C_Q_BLOCK + jnp.arange(C_Q_BLOCK)
        start = (bi * C_Q_BLOCK // C_BLOCK) * C_BLOCK
        k_own = lax.dynamic_slice_in_dim(kp, start, C_BLOCK, axis=2)
        v_own = lax.dynamic_slice_in_dim(vp, start, C_BLOCK, axis=2)
        dist = (qpos[:, None] - (start + blk_off)[None, :]).astype(jnp.float32)
        s_own = jnp.einsum('bhqd,bhkd->bhqk', qi, k_own).astype(jnp.float32) * scale - sl * dist
        scores = [jnp.where(dist >= 0, s_own, NEG)]
        flats = []
        if topk > 0:
            idx, valid = xs[2], xs[3]
            flat = bh_off + idx
            for j in range(topk):
                fj = flat[..., j]
                flats.append(fj)
                kg = kflat[fj]
                kpos_g = idx[..., j][..., None] * C_BLOCK + blk_off
                dist_g = (qpos[None, None, :, None] - kpos_g).astype(jnp.float32)
                sj = jnp.einsum('bhqd,bhqkd->bhqk', qi, kg).astype(jnp.float32) * scale - sl * dist_g
                scores.append(jnp.where(valid[..., j][..., None], sj, NEG))
        w = jax.nn.softmax(jnp.concatenate(scores, axis=-1), axis=-1).astype(dt)
        out = jnp.einsum('bhqk,bhkd->bhqd', w[..., :C_BLOCK], v_own)
        for j, fj in enumerate(flats):
            vg = vflat[fj]
            wj = w[..., (j + 1) * C_BLOCK:(j + 2) * C_BLOCK]
            out = out + jnp.einsum('bhqk,bhqkd->bhqd', wj, vg)
        return out

    o = lax.map(block, xs)
    return merge_blocks(o)


def conv_ffn(x, w_gate, w_up, conv_w, conv_b, w_down):
    hg = x @ w_gate
    hg = lax.conv_general_dilated(hg, conv_w[:, None, :], window_strides=(1,),
                                  padding=[(CONV_W - 1, 0)],
                                  dimension_numbers=('NWC', 'WIO', 'NWC'),
                                  feature_group_count=hg.shape[-1]) + conv_b
    return (jax.nn.gelu(hg, approximate=True) * (x @ w_up)) @ w_down


def setup_inputs(seed: int = 0) -> dict:
    key = jax.random.key(seed)
    ks = jax.random.split(key, 21)
    f32 = jnp.float32

    def nrm(k, shape, scale):
        return jax.random.normal(k, shape, f32) * scale

    return {
        "x": nrm(ks[0], (BATCH, SEQ, D_MODEL), 1.0),
        "p": nrm(ks[1], (DEPTH, BATCH, SEQ, PLE_DIM), 1.0),
        "attn_norm": 1.0 + nrm(ks[2], (DEPTH, D_MODEL), 0.02),
        "w_in": nrm(ks[3], (DEPTH, D_MODEL, IN_COLS), D_MODEL ** -0.5),
        "fox_bf": nrm(ks[4], (DEPTH, B_HEADS), 0.1),
        "lb_logits": nrm(ks[5], (DEPTH, A_W), 0.1),
        "hgrn_onorm": 1.0 + nrm(ks[6], (DEPTH, A_DV), 0.02),
        "fox_qnorm": 1.0 + nrm(ks[7], (DEPTH, HEAD_DIM), 0.02),
        "fox_knorm": 1.0 + nrm(ks[8], (DEPTH, HEAD_DIM), 0.02),
        "moba_qnorm": 1.0 + nrm(ks[9], (DEPTH, HEAD_DIM), 0.02),
        "moba_knorm": 1.0 + nrm(ks[10], (DEPTH, HEAD_DIM), 0.02),
        "w_out": nrm(ks[11], (DEPTH, MIX_W, D_MODEL), MIX_W ** -0.5),
        "ffn_norm": 1.0 + nrm(ks[12], (DEPTH, D_MODEL), 0.02),
        "w_gate": nrm(ks[13], (DEPTH, D_MODEL, D_FF), D_MODEL ** -0.5),
        "w_up": nrm(ks[14], (DEPTH, D_MODEL, D_FF), D_MODEL ** -0.5),
        "conv_w": nrm(ks[15], (DEPTH, CONV_W, D_FF), CONV_W ** -0.5),
        "conv_b": nrm(ks[16], (DEPTH, D_FF), 0.01),
        "w_down": nrm(ks[17], (DEPTH, D_FF, D_MODEL), D_FF ** -0.5),
        "ple_norm": 1.0 + nrm(ks[18], (DEPTH, D_MODEL), 0.02),
        "w_ple_gate": nrm(ks[19], (DEPTH, D_MODEL, D_MODEL), D_MODEL ** -0.5),
        "w_ple_proj": nrm(ks[20], (DEPTH, PLE_DIM, D_MODEL), PLE_DIM ** -0.5),
    }


def reference(x, p, attn_norm, w_in, fox_bf, lb_logits, hgrn_onorm, fox_qnorm, fox_knorm,
              moba_qnorm, moba_knorm, w_out, ffn_norm, w_gate, w_up, conv_w, conv_b, w_down,
              ple_norm, w_ple_gate, w_ple_proj):
    lbs = hgrn2_lower_bounds(lb_logits)
    slopes = alibi_slopes(C_HEADS)
    split_idx = []
    acc = 0
    for n in IN_SPLITS[:-1]:
        acc += n
        split_idx.append(acc)
    h = x
    for i in range(DEPTH):
        a = rmsnorm(h, attn_norm[i])
        proj = a @ w_in[i]
        aq, af, ai, ag, bq, bk, bv, bf, cq, ck, cv = jnp.split(proj, split_idx, axis=-1)
        oa = hgrn2_mixer(aq, af, ai, ag, lbs[i], hgrn_onorm[i])
        ob = fox_mixer(bq, bk, bv, bf, fox_bf[i], fox_qnorm[i], fox_knorm[i])
        oc = moba_mixer(cq, ck, cv, moba_qnorm[i], moba_knorm[i], slopes)
        h = h + jnp.concatenate([oa, ob, oc], axis=-1) @ w_out[i]
        c = rmsnorm(h, ffn_norm[i])
        h = h + conv_ffn(c, w_gate[i], w_up[i], conv_w[i], conv_b[i], w_down[i])
        e = rmsnorm(h, ple_norm[i])
        h = h + jax.nn.sigmoid(e @ w_ple_gate[i]) * (p[i] @ w_ple_proj[i])
    return h
```

```python
import os
from contextlib import ExitStack
import numpy as np
import concourse.bass as bass
import concourse.mybir as mybir
from concourse.bass_utils import run_bass_kernel_spmd

F32 = mybir.dt.float32
BF16 = mybir.dt.bfloat16
AF = mybir.ActivationFunctionType
ALU = mybir.AluOpType
AX = mybir.AxisListType

D = 2048
KC = 16
DFF = 5632
FC = 44
PLE = 256
INC = 6662
EPS = 1e-6
NEGM = -1e30
SELB = 30000.0
COL = dict(aq=0, af=512, ai=1024, ag=1536, bq=2048, bk=2816, bv=3584, bf=4352, cq=4358, ck=5126, cv=5894)
NDSEM = 40


class Res:
    __slots__ = ("name", "w", "r")

    def __init__(self, name):
        self.name = name
        self.w = None
        self.r = {}


class FW:
    ENGS = ("pe", "act", "dve", "pool", "sp")

    def __init__(self, nc, es):
        self.nc = nc
        self.csem = {e: es.enter_context(nc.semaphore("c_" + e)) for e in ("pe", "act", "dve", "pool")}
        self.ccount = {e: 0 for e in self.csem}
        self.dsem = [es.enter_context(nc.semaphore(f"d{i}")) for i in range(NDSEM)]
        self.dcount = [0] * NDSEM
        self.dnext = {"pool": 0, "sp": 8}
        self.drange = {"pool": (0, 3), "sp": (8, NDSEM)}
        self.seen = {e: {} for e in self.ENGS}
        self.res = {}
        self.engobj = {"pe": nc.tensor, "act": nc.scalar, "dve": nc.vector, "pool": nc.gpsimd, "sp": nc.sync}
        self.ninst = 0

    def R(self, *key):
        r = self.res.get(key)
        if r is None:
            r = self.res[key] = Res(key)
        return r

    def _wait(self, eng, key, n):
        if key == eng and eng == "pe":
            return
        if self.seen[eng].get(key, 0) >= n:
            return
        self.seen[eng][key] = n
        sem = self.csem[key] if isinstance(key, str) else self.dsem[key]
        self.engobj[eng].wait_ge(sem, n)
        self.ninst += 1

    def _deps(self, eng, reads, writes):
        for r in reads:
            if r.w is not None:
                self._wait(eng, *r.w)
        for w in writes:
            if w.w is not None:
                self._wait(eng, *w.w)
            for k, n in w.r.items():
                self._wait(eng, k, n)

    def _commit(self, tok, reads, writes):
        k, n = tok
        for r in reads:
            if r.r.get(k, 0) < n:
                r.r[k] = n
        for w in writes:
            w.w = tok
            w.r = {}

    def op(self, eng, fn, reads=(), writes=()):
        self._deps(eng, reads, writes)
        self.ccount[eng] += 1
        fn(self.engobj[eng]).then_inc(self.csem[eng], 1)
        self.ninst += 1
        self._commit((eng, self.ccount[eng]), reads, writes)

    def dma(self, out, in_, reads=(), writes=(), q="sp", **kw):
        self._deps(q, reads, writes)
        i = self.dnext[q]
        lo, hi = self.drange[q]
        self.dnext[q] = lo + (i + 1 - lo) % (hi - lo)
        if self.dcount[i] > 0:
            self._wait(q, i, self.dcount[i])
        self.dcount[i] += 16
        self.engobj[q].dma_start(out=out, in_=in_, **kw).then_inc(self.dsem[i], 16)
        self.ninst += 1
        self._commit((i, self.dcount[i]), reads, writes)

    def barrier(self, all_dma=False):
        lo = 0 if all_dma else self.drange["pool"][1]
        for e in self.ENGS:
            for e2 in self.csem:
                if e2 != e and self.ccount[e2] > 0:
                    self._wait(e, e2, self.ccount[e2])
            for i in range(lo, NDSEM):
                if self.dcount[i] > 0:
                    self._wait(e, i, self.dcount[i])


def build(S, L, dbg=()):
    NT = S // 512
    NB = S // 128
    NCH = S // 64
    TA = min(S, 2048)
    nc = bass.Bass("TRN2", target_bir_lowering=False)

    def ext_in(name, shape, dt=F32):
        return nc.dram_tensor(name, list(shape), dt, kind="ExternalInput").ap()

    def scratch(name, shape, dt):
        kind = "ExternalOutput" if name in dbg else "Internal"
        return nc.dram_tensor(name, list(shape), dt, kind=kind).ap()

    xT = ext_in("xT", [D, S])
    pT = ext_in("pT", [L, PLE, S])
    W32 = dict(
        w_in=ext_in("w_in", [L, D, INC]), w_out=ext_in("w_out", [L, D, D]),
        w_gate=ext_in("w_gate", [L, D, DFF]), w_up=ext_in("w_up", [L, D, DFF]),
        w_down=ext_in("w_down", [L, DFF, D]), w_pg=ext_in("w_pg", [L, D, D]),
        w_pp=ext_in("w_pp", [L, PLE, D]))
    gA_d = ext_in("gA", [128, L, KC]); gF_d = ext_in("gF", [128, L, KC]); gP_d = ext_in("gP", [128, L, KC])
    convw_d = ext_in("convw", [128, L, 3, FC]); convb_d = ext_in("convb", [128, L, FC])
    lbl_d = ext_in("lbl", [128, L, 4])
    hv_d = ext_in("hv", [128, 5, L])
    fbf_d = ext_in("fbf", [6, L])
    ident_d = ext_in("ident", [128, 128])
    cmask_d = ext_in("cmask", [128, 4, 512])
    hmask_d = ext_in("hmask", [64, 64])
    qterm_d = ext_in("qterm", [128, 6, 512])
    kcol_d = ext_in("kcol", [128, 6, 40])
    ind_d = ext_in("ind", [32, 16, 128])
    pastm_d = ext_in("pastm", [128, 32, 16])
    ownm_d = ext_in("ownm", [128, 32, 16])
    outT = nc.dram_tensor("outT", [D, S], F32, kind="ExternalOutput").ap()

    hbuf = scratch("hbuf", [D, S], F32)
    Wb = {k: scratch("b_" + k, v.shape, BF16) for k, v in W32.items()}
    hg_q = scratch("hg_q", [4, 128, S], BF16); hg_k = scratch("hg_k", [4, 128, S], BF16)
    hg_g = scratch("hg_g", [4, 128, S], BF16); hg_lf = scratch("hg_lf", [4, 128, S], F32)
    hg_v = scratch("hg_v", [S, 512], BF16)
    fx_q = scratch("fx_q", [6, 128, S], BF16); fx_k = scratch("fx_k", [6, 128, S], BF16)
    fx_v = scratch("fx_v", [S, 768], BF16); fx_f = scratch("fx_f", [6, S], F32); fx_fc = scratch("fx_fc", [6, S], F32)
    mb_q = scratch("mb_q", [6, 128, S], BF16); mb_k = scratch("mb_k", [6, 128, S], BF16)
    mb_v = scratch("mb_v", [S, 768], BF16)
    mixT = scratch("mixT", [D, S], BF16)

    es = ExitStack()
    with es:
        fw = FW(nc, es)
        R = fw.R

        uid = [0]

        def T(st, name, shape, dt=F32):
            uid[0] += 1
            return st.enter_context(nc.sbuf_tensor(f"s{uid[0]}_{name}", list(shape), dt))

        P = [es.enter_context(nc.psum_tensor(f"P{i}", [128, 512], F32)) for i in range(7)]
        PB = es.enter_context(nc.psum_tensor("PB", [128, 1024], BF16))
        RP = [R("P", i) for i in range(7)]
        RPB = [R("PB", 0), R("PB", 1)]

        cst = es
        gA = T(cst, "gA", [128, L, KC]); gF = T(cst, "gF", [128, L, KC]); gP = T(cst, "gP", [128, L, KC])
        convw = T(cst, "convw", [128, L, 3, FC]); convb = T(cst, "convb", [128, L, FC])
        lbl = T(cst, "lbl", [128, L, 4]); hv = T(cst, "hv", [128, 5, L]); fbf = T(cst, "fbf", [6, L])
        identf = T(cst, "identf", [128, 128]); identb = T(cst, "identb", [128, 128], BF16)
        hmask = T(cst, "hmask", [64, 64])
        gate_t = T(cst, "gate_t", [128, 8])
        indb = T(cst, "indb", [32, 16, 128], BF16)
        onesD = T(cst, "onesD", [128, 128], BF16); onesH = T(cst, "onesH", [128, 128], BF16)
        ones1 = T(cst, "ones1", [128, 128], BF16)
        lb = T(cst, "lb", [128, L, 4]); oml = T(cst, "oml", [128, L, 4]); noml = T(cst, "noml", [128, L, 4])
        lbe = T(cst, "lbe", [128, L, 4]); lbs = T(cst, "lbs", [128, 4])
        gqs = T(cst, "gqs", [128, 2, L])
        rc = R("consts")
        for dst, src in ((gA, gA_d), (gF, gF_d), (gP, gP_d), (convw, convw_d), (convb, convb_d), (lbl, lbl_d),
                         (hv, hv_d), (fbf, fbf_d), (identf, ident_d), (hmask, hmask_d)):
            fw.dma(dst[:], src, writes=[R("c", len(fw.res))])
        fw.dma(indb[:], ind_d, writes=[R("c", len(fw.res))], q="pool")
        fw.barrier(all_dma=True)
        fw.op("dve", lambda e: e.tensor_copy(identb[:], identf[:]), writes=[rc])
        fw.op("pool", lambda e: e.memset(onesD[:], 1.0 / D), writes=[rc])
        fw.op("pool", lambda e: e.memset(onesH[:], 1.0 / 128), writes=[rc])
        fw.op("pool", lambda e: e.memset(ones1[:], 1.0), writes=[rc])
        fw.op("act", lambda e: e.activation(lbe[:], lbl[:], AF.Exp), writes=[rc])
        fw.barrier()
        fw.op("dve", lambda e: e.tensor_copy(lbs[:], lbe[:, 0, :]), writes=[rc])
        for l in range(1, L):
            fw.op("dve", lambda e, l=l: e.tensor_tensor(lbs[:], lbs[:], lbe[:, l, :], ALU.add), writes=[rc])
        fw.op("dve", lambda e: e.reciprocal(lbs[:], lbs[:]), writes=[rc])
        for l in range(L):
            fw.op("dve", lambda e, l=l: e.tensor_tensor(lbe[:, l, :], lbe[:, l, :], lbs[:], ALU.mult), writes=[rc])
        fw.op("dve", lambda e: e.memset(lb[:, 0, :], 0.0), writes=[rc])
        for l in range(1, L):
            fw.op("dve", lambda e, l=l: e.tensor_tensor(lb[:, l, :], lb[:, l - 1, :], lbe[:, l, :], ALU.add), writes=[rc])
        fw.op("dve", lambda e: e.tensor_scalar(oml[:], lb[:], -1.0, 1.0, ALU.mult, ALU.add), writes=[rc])
        fw.op("dve", lambda e: e.tensor_scalar(noml[:], lb[:], 1.0, -1.0, ALU.mult, ALU.add), writes=[rc])
        fw.op("dve", lambda e: e.tensor_scalar(gqs[:, 0, :], hv[:, 1, :], 128 ** -0.5, 0.0, ALU.mult, ALU.add), writes=[rc])
        fw.op("dve", lambda e: e.tensor_scalar(gqs[:, 1, :], hv[:, 3, :], 128 ** -0.5, 0.0, ALU.mult, ALU.add), writes=[rc])
        fw.barrier()

        A_F = [("aq", 4), ("af", 4), ("ag", 4), ("bq", 6), ("bk", 6), ("cq", 6), ("ck", 6)]
        A_T = [("ai", 512), ("bv", 768), ("cv", 768)]
        A_ORDER = [COL[nm] + pc * 256 for nm, nh in A_F for pc in range(nh // 2)] + \
                  [COL[nm] + pc * 256 for nm, nc_ in A_T for pc in range(nc_ // 256)]

        def cast_weights(l, gate=None):
            src = W32["w_in"]
            g = [gate] if gate is not None else []
            for col0 in A_ORDER[:6] + [COL["bf"]] + A_ORDER[6:]:
                ncols = 6 if col0 == COL["bf"] else 256
                fw.dma(Wb["w_in"][l, :, col0:col0 + ncols], src[l, :, col0:col0 + ncols],
                       reads=g, writes=[R("wA", l, col0)], q="pool")
            for k, src in W32.items():
                if k == "w_in":
                    continue
                rows = src.shape[1]
                for rb in range(rows // 64):
                    fw.dma(Wb[k][l, rb * 64:(rb + 1) * 64, :], src[l, rb * 64:(rb + 1) * 64, :],
                           writes=[R("w", k, l, rb)], q="pool")

        def wres(k, l):
            return [R("w", k, l, rb) for rb in range(W32[k].shape[1] // 64)]

        def rmsnorm_tile(st, hin, gain, l, out_fn, rin, rout, tag):
            sq = [st[f"sq{i}"] for i in range(2)]
            for c in range(KC):
                b = c % 2
                fw.op("act", lambda e, c=c, b=b: e.activation(sq[b][:], hin[:, c, :], AF.Square),
                      reads=[rin], writes=[R(tag, "sq", b)])
                fw.op("pe", lambda e, c=c, b=b: e.matmul(P[6][:], onesD[:], sq[b][:], start=(c == 0), stop=(c == KC - 1)),
                      reads=[R(tag, "sq", b)], writes=[RP[6]])
            rs, rstd = st["rs"], st["rstd"]
            fw.op("act", lambda e: e.activation(rs[:], P[6][:], AF.Ln, bias=EPS), reads=[RP[6]], writes=[R(tag, "rs")])
            fw.op("act", lambda e: e.activation(rstd[:], rs[:], AF.Exp, scale=-0.5), reads=[R(tag, "rs")], writes=[R(tag, "rstd")])
            for c in range(KC):
                fw.op("dve", lambda e, c=c: e.scalar_tensor_tensor(out_fn(c), hin[:, c, :], gain[:, l, c:c + 1], rstd[:],
                                                                     ALU.mult, ALU.mult),
                      reads=[rin, R(tag, "rstd")], writes=[rout(c)])

        def phase_A(l):
            hsrc = xT if l == 0 else hbuf
            hv3 = hsrc.rearrange("(c p) s -> p c s", p=128)
            with ExitStack() as st_:
                aT = T(st_, "aT", [128, KC, TA], BF16)
                hins = [T(st_, f"hin{i}", [128, KC, 512]) for i in range(2)]
                st = dict(sq0=T(st_, "sq0", [128, 512], BF16), sq1=T(st_, "sq1", [128, 512], BF16),
                          rs=T(st_, "rs", [128, 512]), rstd=T(st_, "rstd", [128, 512]))
                NWP = 4
                wp = [T(st_, f"wp{i}", [128, KC, 256], BF16) for i in range(NWP)]
                wf = T(st_, "wf", [128, KC, 6], BF16)
                ob = [T(st_, f"ob{i}", [128, 512], BF16) for i in range(3)]
                of = [T(st_, f"of{i}", [128, 512]) for i in range(2)]
                s1 = [T(st_, f"s1{i}", [128, 512]) for i in range(2)]
                s2 = [T(st_, f"s2{i}", [128, 512]) for i in range(2)]
                qsq = [T(st_, f"qsq{i}", [128, 512], BF16) for i in range(2)]
                qrs = [T(st_, f"qrs{i}", [128, 512]) for i in range(2)]
                vb = [T(st_, f"vb{i}", [128, 256], BF16) for i in range(2)]
                cnt = dict(p=0, ob=0, of=0, q=0, wp=0, vb=0)

                pending = []
                seq = A_ORDER * (S // TA)
                issued = [0]

                def load_panel(col0):
                    n = cnt["wp"]
                    cnt["wp"] += 1
                    assert seq[n] == col0
                    while issued[0] < min(len(seq), n + NWP - 1):
                        m = issued[0]
                        issued[0] += 1
                        src = Wb["w_in"][l].rearrange("(c p) n -> p c n", p=128)[:, :, seq[m]:seq[m] + 256]
                        fw.dma(wp[m % NWP][:], src, reads=[R("wA", l, seq[m])], writes=[R("A", "wp", m % NWP)])
                    return n % NWP

                F_SEGS = [("aq", 4, "silu", hg_q), ("af", 4, "hf", None), ("ag", 4, "silu", hg_g),
                          ("bq", 6, "nq", fx_q), ("bk", 6, "nk", fx_k), ("cq", 6, "mq", mb_q), ("ck", 6, "mk", mb_k)]
                for half in range(S // TA):
                    nsub = TA // 512
                    for sub in range(nsub):
                        t = half * nsub + sub
                        hin = hins[t % 2]
                        if sub == 0:
                            fw.dma(hin[:], hv3[:, :, t * 512:(t + 1) * 512], reads=[R("h", t)], writes=[R("A", "hin", t % 2)])
                        if sub + 1 < nsub:
                            fw.dma(hins[(t + 1) % 2][:], hv3[:, :, (t + 1) * 512:(t + 2) * 512], reads=[R("h", t + 1)],
                                   writes=[R("A", "hin", (t + 1) % 2)])
                        rmsnorm_tile(st, hin, gA, l, lambda c, sub=sub: aT[:, c, sub * 512:(sub + 1) * 512],
                                     R("A", "hin", t % 2), (lambda c, sub=sub: R("A", "aT", sub, c)), "An")
                    for (nm, nh, epi, dst) in F_SEGS:
                        for pc in range(nh // 2):
                            wi = load_panel(COL[nm] + pc * 256)
                            for sub in range(nsub):
                                t = half * nsub + sub
                                tsl = slice(t * 512, (t + 1) * 512)
                                for g in range(2):
                                    head = pc * 2 + g
                                    pb = (0, 1, 4, 5)[cnt["p"] % 4]
                                    cnt["p"] += 1
                                    for kc in range(KC):
                                        fw.op("pe", lambda e, kc=kc, g=g, wi=wi, pb=pb, sub=sub: e.matmul(
                                            P[pb][:], wp[wi][:, kc, g * 128:(g + 1) * 128], aT[:, kc, sub * 512:(sub + 1) * 512],
                                            start=(kc == 0), stop=(kc == KC - 1)),
                                            reads=[R("A", "wp", wi), R("A", "aT", sub, kc)], writes=[RP[pb]])
                                    while pending:
                                        pending.pop(0)()
                                    if epi == "silu":
                                        oi = cnt["ob"] % 3
                                        cnt["ob"] += 1
                                        fw.op("act", lambda e, pb=pb, oi=oi: e.activation(ob[oi][:], P[pb][:], AF.Silu),
                                              reads=[RP[pb]], writes=[R("A", "ob", oi)])
                                        fw.dma(dst[head, :, tsl], ob[oi][:], reads=[R("A", "ob", oi)], writes=[R(nm, head, t)])
                                    elif epi == "hf":
                                        si = cnt["of"] % 2
                                        cnt["of"] += 1
                                        oi = cnt["ob"] % 3
                                        cnt["ob"] += 1
                                        fw.op("act", lambda e, pb=pb, si=si: e.activation(s1[si][:], P[pb][:], AF.Sigmoid),
                                              reads=[RP[pb]], writes=[R("A", "s1", si)])
                                        fw.op("dve", lambda e, si=si, oi=oi, head=head: e.tensor_scalar(
                                            ob[oi][:], s1[si][:], noml[:, l, head:head + 1], oml[:, l, head:head + 1], ALU.mult, ALU.add),
                                            reads=[R("A", "s1", si)], writes=[R("A", "ob", oi)])
                                        fw.dma(hg_k[head, :, tsl], ob[oi][:], reads=[R("A", "ob", oi)], writes=[R("hk", head, t)])
                                        fw.op("dve", lambda e, si=si, head=head: e.tensor_scalar(
                                            s2[si][:], s1[si][:], oml[:, l, head:head + 1], lb[:, l, head:head + 1], ALU.mult, ALU.add),
                                            reads=[R("A", "s1", si)], writes=[R("A", "s2", si)])
                                        fw.op("act", lambda e, si=si: e.activation(of[si][:], s2[si][:], AF.Ln),
                                              reads=[R("A", "s2", si)], writes=[R("A", "of", si)])
                                        fw.dma(hg_lf[head, :, tsl], of[si][:], reads=[R("A", "of", si)], writes=[R("hlf", head, t)])
                                    else:
                                        gain = {"nq": gqs[:, 0, l:l + 1], "nk": hv[:, 2, l:l + 1],
                                                "mq": gqs[:, 1, l:l + 1], "mk": hv[:, 4, l:l + 1]}[epi]
                                        qi = cnt["q"] % 2
                                        cnt["q"] += 1
                                        oi = cnt["ob"] % 3
                                        cnt["ob"] += 1
                                        fw.op("act", lambda e, pb=pb, qi=qi: e.activation(qsq[qi][:], P[pb][:], AF.Square),
                                              reads=[RP[pb]], writes=[R("A", "qsq", qi)])
                                        def tail(pb=pb, qi=qi, oi=oi, gain=gain, head=head, tsl=tsl, t=t, dst=dst, nm=nm):
                                            fw.op("pe", lambda e: e.matmul(P[2 + qi][:], onesH[:], qsq[qi][:], start=True, stop=True),
                                                  reads=[R("A", "qsq", qi)], writes=[RP[2 + qi]])
                                            fw.op("act", lambda e: e.activation(qrs[qi][:], P[2 + qi][:], AF.Ln, bias=EPS),
                                                  reads=[RP[2 + qi]], writes=[R("A", "qrs", qi)])
                                            fw.op("act", lambda e: e.activation(qrs[qi][:], qrs[qi][:], AF.Exp, scale=-0.5),
                                                  reads=[R("A", "qrs", qi)], writes=[R("A", "qrs", qi)])
                                            fw.op("dve", lambda e: e.scalar_tensor_tensor(
                                                ob[oi][:], P[pb][:], gain, qrs[qi][:], ALU.mult, ALU.mult),
                                                reads=[RP[pb], R("A", "qrs", qi)], writes=[R("A", "ob", oi)])
                                            fw.dma(dst[head, :, tsl], ob[oi][:], reads=[R("A", "ob", oi)], writes=[R(nm, head, t)])
                                        pending.append(tail)
                    while pending:
                        pending.pop(0)()
                    srcf = Wb["w_in"][l].rearrange("(c p) n -> p c n", p=128)[:, :, COL["bf"]:COL["bf"] + 6]
                    fw.dma(wf[:], srcf, reads=[R("wA", l, COL["bf"])], writes=[R("A", "wf")])
                    for sub in range(nsub):
                        t = half * nsub + sub
                        pb = (0, 1, 4, 5)[cnt["p"] % 4]
                        cnt["p"] += 1
                        for kc in range(KC):
                            fw.op("pe", lambda e, kc=kc, pb=pb, sub=sub: e.matmul(
                                P[pb][0:6, :], wf[:, kc, :], aT[:, kc, sub * 512:(sub + 1) * 512],
                                start=(kc == 0), stop=(kc == KC - 1)),
                                reads=[R("A", "wf"), R("A", "aT", sub, kc)], writes=[RP[pb]])
                        si = cnt["of"] % 2
                        cnt["of"] += 1
                        fw.op("act", lambda e, pb=pb, si=si: e.activation(s1[si][0:6, :], P[pb][0:6, :], AF.Sigmoid, bias=fbf[:, l:l + 1]),
                              reads=[RP[pb]], writes=[R("A", "s1", si)])
                        fw.op("act", lambda e, si=si: e.activation(of[si][0:6, :], s1[si][0:6, :], AF.Ln),
                              reads=[R("A", "s1", si)], writes=[R("A", "of", si)])
                        fw.dma(fx_f[:, t * 512:(t + 1) * 512], of[si][0:6, :], reads=[R("A", "of", si)], writes=[R("ff", t)])
                    for (nm, ncols, dst) in (("ai", 512, hg_v), ("bv", 768, fx_v), ("cv", 768, mb_v)):
                        for pc in range(ncols // 256):
                            wi = load_panel(COL[nm] + pc * 256)
                            for tb in range(TA // 128):
                                tbg = half * (TA // 128) + tb
                                sub = tb // 4
                                pb = (0, 1, 4, 5)[cnt["p"] % 4]
                                cnt["p"] += 1
                                for kc in range(KC):
                                    fw.op("pe", lambda e, kc=kc, wi=wi, pb=pb, tb=tb: e.matmul(
                                        P[pb][:, 0:256], aT[:, kc, tb * 128:(tb + 1) * 128], wp[wi][:, kc, :],
                                        start=(kc == 0), stop=(kc == KC - 1)),
                                        reads=[R("A", "wp", wi), R("A", "aT", sub, kc)], writes=[RP[pb]])
                                vi = cnt["vb"] % 2
                                cnt["vb"] += 1
                                eng = "act" if vi == 0 else "dve"
                                if eng == "act":
                                    fw.op("act", lambda e, pb=pb, vi=vi: e.copy(vb[vi][:], P[pb][:, 0:256]),
                                          reads=[RP[pb]], writes=[R("A", "vb", vi)])
                                else:
                                    fw.op("dve", lambda e, pb=pb, vi=vi: e.tensor_copy(vb[vi][:], P[pb][:, 0:256]),
                                          reads=[RP[pb]], writes=[R("A", "vb", vi)])
                                fw.dma(dst[tbg * 128:(tbg + 1) * 128, pc * 256:(pc + 1) * 256], vb[vi][:],
                                       reads=[R("A", "vb", vi)], writes=[R(nm, tbg, pc)])
            fw.barrier()

        def phase_hgrn(l):
            for h in range(4):
                with ExitStack() as st_:
                    q = T(st_, "hq", [128, S], BF16); k = T(st_, "hk", [128, S], BF16); g = T(st_, "hgt", [128, S], BF16)
                    cs = [T(st_, f"cs{i}", [128, S]) for i in range(2)]
                    E = T(st_, "hE", [128, S])
                    qd = T(st_, "qd", [128, S], BF16); kd = T(st_, "kd", [128, S], BF16)
                    qe = T(st_, "qe", [128, S], BF16); klT = T(st_, "klT", [128, S], BF16)
                    v = T(st_, "hv_", [64, NCH, 128], BF16)
                    oraw = T(st_, "oraw", [128, S])
                    ebl = T(st_, "ebl", [128, NCH])
                    stf = T(st_, "stf", [128, 128]); stb = [T(st_, f"stb{i}", [128, 128], BF16) for i in range(2)]
                    asb = [T(st_, f"asb{i}", [64, 64], BF16) for i in range(2)]
                    klsb = [T(st_, f"klsb{i}", [64, 128], BF16) for i in range(2)]
                    sq = T(st_, "hsq", [128, 512], BF16); rs = T(st_, "hrs", [128, 512]); y = T(st_, "hy", [128, 512])
                    yo = [T(st_, f"hyo{i}", [128, 512], BF16) for i in range(2)]
                    fw.dma(q[:], hg_q[h], reads=[R("aq", h, t) for t in range(NT)], writes=[R("H", "q")])
                    fw.dma(k[:], hg_k[h], reads=[R("hk", h, t) for t in range(NT)], writes=[R("H", "k")])
                    fw.dma(g[:], hg_g[h], reads=[R("ag", h, t) for t in range(NT)], writes=[R("H", "g")])
                    fw.dma(cs[0][:], hg_lf[h], reads=[R("hlf", h, t) for t in range(NT)], writes=[R("H", "cs", 0, "A"), R("H", "cs", 0, "C")])
                    fw.dma(v[:], hg_v.rearrange("(c s) n -> s c n", s=64)[:, :, h * 128:(h + 1) * 128],
                           reads=[R("ai", tb, pc) for tb in range(NB) for pc in range(2)], writes=[R("H", "v")])
                    cur = 0
                    for d in (1, 2, 4, 8, 16, 32):
                        a3 = cs[cur][:].rearrange("p (c t) -> p c t", t=64)
                        b3 = cs[1 - cur][:].rearrange("p (c t) -> p c t", t=64)
                        rd = [R("H", "cs", cur, "A"), R("H", "cs", cur, "C")]
                        fw.op("dve", lambda e, a3=a3, b3=b3, d=d: e.tensor_tensor(b3[:, :, d:64], a3[:, :, d:64], a3[:, :, 0:64 - d], ALU.add),
                              reads=rd, writes=[R("H", "cs", 1 - cur, "A")])
                        fw.op("pool", lambda e, a3=a3, b3=b3, d=d: e.tensor_copy(b3[:, :, 0:d], a3[:, :, 0:d]),
                              reads=rd, writes=[R("H", "cs", 1 - cur, "C")])
                        cur = 1 - cur
                    bb = cs[cur]
                    tmp = cs[1 - cur]
                    rb_ = [R("H", "cs", cur, "A"), R("H", "cs", cur, "C")]
                    rt_ = [R("H", "cs", 1 - cur, "A"), R("H", "cs", 1 - cur, "C")]
                    b3 = bb[:].rearrange("p (c t) -> p c t", t=64)
                    t3 = tmp[:].rearrange("p (c t) -> p c t", t=64)
                    fw.op("dve", lambda e: e.tensor_tensor(t3, b3, b3[:, :, 31:32].broadcast_to([128, NCH, 64]), ALU.subtract),
                          reads=rb_, writes=rt_)
                    fw.op("act", lambda e: e.activation(E[:], tmp[:], AF.Exp), reads=rt_, writes=[R("H", "E")])
                    fw.op("dve", lambda e: e.tensor_tensor(qd[:], q[:], E[:], ALU.mult), reads=[R("H", "q"), R("H", "E")], writes=[R("H", "qd")])
                    fw.op("act", lambda e: e.activation(E[:], tmp[:], AF.Exp, scale=-1.0), reads=rt_, writes=[R("H", "E")])
                    fw.op("dve", lambda e: e.tensor_tensor(kd[:], k[:], E[:], ALU.mult), reads=[R("H", "k"), R("H", "E")], writes=[R("H", "kd")])
                    fw.op("act", lambda e: e.activation(E[:], bb[:], AF.Exp), reads=rb_, writes=[R("H", "E")])
                    fw.op("dve", lambda e: e.tensor_tensor(qe[:], q[:], E[:], ALU.mult), reads=[R("H", "q"), R("H", "E")], writes=[R("H", "qe")])
                    fw.op("dve", lambda e: e.tensor_tensor(t3, b3, b3[:, :, 63:64].broadcast_to([128, NCH, 64]), ALU.subtract),
                          reads=rb_, writes=rt_)
                    fw.op("act", lambda e: e.activation(E[:], tmp[:], AF.Exp, scale=-1.0), reads=rt_, writes=[R("H", "E")])
                    fw.op("dve", lambda e: e.tensor_tensor(klT[:], k[:], E[:], ALU.mult), reads=[R("H", "k"), R("H", "E")], writes=[R("H", "klT")])
                    fw.op("act", lambda e: e.activation(ebl[:].rearrange("p (c o) -> p c o", o=1), b3[:, :, 63:64], AF.Exp), reads=rb_, writes=[R("H", "ebl")])
                    fw.op("dve", lambda e: e.memset(stf[:], 0.0), writes=[R("H", "stf")])
                    fw.op("dve", lambda e: e.memset(stb[0][:], 0.0), writes=[R("H", "stb", 0)])
                    for c in range(NCH):
                        csl = slice(c * 64, (c + 1) * 64)
                        pa = c % 2
                        fw.op("pe", lambda e, csl=csl, pa=pa: e.matmul(P[pa][0:64, 0:64], kd[:, csl], qd[:, csl], start=True, stop=True),
                              reads=[R("H", "kd"), R("H", "qd")], writes=[RP[pa]])
                        fw.op("dve", lambda e, pa=pa: e.tensor_tensor(asb[pa][:], P[pa][0:64, 0:64], hmask[:], ALU.mult),
                              reads=[RP[pa]], writes=[R("H", "asb", pa)])
                        fw.op("pe", lambda e, csl=csl, pa=pa: e.transpose(PB[0:64, pa * 128:(pa + 1) * 128], klT[:, csl], identb[:]),
                              reads=[R("H", "klT")], writes=[RPB[pa]])
                        fw.op("act", lambda e, pa=pa: e.copy(klsb[pa][:], PB[0:64, pa * 128:(pa + 1) * 128]),
                              reads=[RPB[pa]], writes=[R("H", "klsb", pa)])
                        fw.op("pe", lambda e, c=c, pa=pa: e.matmul(P[4 + pa][:, 0:128], klsb[pa][:], v[:, c, :], start=True, stop=True),
                              reads=[R("H", "klsb", pa), R("H", "v")], writes=[RP[4 + pa]])
                        fw.op("dve", lambda e, c=c, pa=pa: e.scalar_tensor_tensor(stf[:], stf[:], ebl[:, c:c + 1], P[4 + pa][:, 0:128], ALU.mult, ALU.add),
                              reads=[RP[4 + pa], R("H", "ebl")], writes=[R("H", "stf")])
                        fw.op("act", lambda e, pa=pa: e.copy(stb[1 - pa][:], stf[:]),
                              reads=[R("H", "stf")], writes=[R("H", "stb", 1 - pa)])
                        fw.op("pe", lambda e, csl=csl, pa=pa: e.matmul(P[2 + pa][:, 0:64], stb[pa][:], qe[:, csl], start=True, stop=False),
                              reads=[R("H", "stb", pa), R("H", "qe")], writes=[RP[2 + pa]])
                        fw.op("pe", lambda e, c=c, pa=pa: e.matmul(P[2 + pa][:, 0:64], v[:, c, :], asb[pa][:], start=False, stop=True),
                              reads=[R("H", "v"), R("H", "asb", pa)], writes=[RP[2 + pa]])
                        fw.op("act", lambda e, csl=csl, pa=pa: e.copy(oraw[:, csl], P[2 + pa][:, 0:64]),
                              reads=[RP[2 + pa]], writes=[R("H", "oraw", c // 8)])
                    for t in range(NT):
                        tsl = slice(t * 512, (t + 1) * 512)
                        fw.op("act", lambda e, tsl=tsl: e.activation(sq[:], oraw[:, tsl], AF.Square), reads=[R("H", "oraw", t)], writes=[R("H", "sq")])
                        fw.op("pe", lambda e: e.matmul(P[6][:], onesH[:], sq[:], start=True, stop=True), reads=[R("H", "sq")], writes=[RP[6]])
                        fw.op("act", lambda e: e.activation(rs[:], P[6][:], AF.Ln, bias=EPS), reads=[RP[6]], writes=[R("H", "rs")])
                        fw.op("act", lambda e: e.activation(rs[:], rs[:], AF.Exp, scale=-0.5), reads=[R("H", "rs")], writes=[R("H", "rs")])
                        fw.op("dve", lambda e, tsl=tsl: e.scalar_tensor_tensor(y[:], oraw[:, tsl], hv[:, 0, l:l + 1], rs[:], ALU.mult, ALU.mult),
                              reads=[R("H", "oraw", t), R("H", "rs")], writes=[R("H", "y")])
                        yi = t % 2
                        fw.op("dve", lambda e, tsl=tsl, yi=yi: e.tensor_tensor(yo[yi][:], y[:], g[:, tsl], ALU.mult),
                              reads=[R("H", "y"), R("H", "g")], writes=[R("H", "yo", yi)])
                        fw.dma(mixT[h * 128:(h + 1) * 128, tsl], yo[yi][:], reads=[R("H", "yo", yi)], writes=[R("mix", h, t)])
                fw.barrier()

        def phase_attn(l):
            with ExitStack() as sl_:
                nfk = T(sl_, "nfk", [128, NB, 6])
                qterm = T(sl_, "qterm", [128, 6, 512]); kcol = T(sl_, "kcol", [128, 6, 40])
                pastm = T(sl_, "pastm", [128, 32, 16]); ownm = T(sl_, "ownm", [128, 32, 16])
                cmaskb = T(sl_, "cmaskb", [128, 4, 512], BF16)
                AC = dict(cmaskb=cmaskb, qterm=qterm, kcol=kcol, indb=indb, pastm=pastm, ownm=ownm)
                with ExitStack() as sf_:
                    cmask = T(sf_, "cmask", [128, 4, 512])
                    fc = [T(sf_, f"fc{i}", [6, S]) for i in range(2)]
                    for dst, src in ((cmask, cmask_d), (qterm, qterm_d), (kcol, kcol_d), (pastm, pastm_d), (ownm, ownm_d)):
                        fw.dma(dst[:], src, writes=[R("X", "c", len(fw.res))])
                    fw.barrier()
                    fw.op("dve", lambda e: e.tensor_scalar(cmaskb[:], cmask[:], -SELB / NEGM, 0.0, ALU.mult, ALU.add), writes=[R("X", "cmaskb")])
                    fw.dma(fc[0][:], fx_f, reads=[R("ff", t) for t in range(NT)], writes=[R("X", "fc", 0, "A"), R("X", "fc", 0, "C")])
                    cur = 0
                    d = 1
                    while d < S:
                        rd = [R("X", "fc", cur, "A"), R("X", "fc", cur, "C")]
                        fw.op("dve", lambda e, cur=cur, d=d: e.tensor_tensor(fc[1 - cur][:, d:S], fc[cur][:, d:S], fc[cur][:, 0:S - d], ALU.add),
                              reads=rd, writes=[R("X", "fc", 1 - cur, "A")])
                        fw.op("pool", lambda e, cur=cur, d=d: e.tensor_copy(fc[1 - cur][:, 0:d], fc[cur][:, 0:d]),
                              reads=rd, writes=[R("X", "fc", 1 - cur, "C")])
                        cur = 1 - cur
                        d *= 2
                    rfc = [R("X", "fc", cur, "A"), R("X", "fc", cur, "C")]
                    fw.dma(fx_fc, fc[cur][:], reads=rfc, writes=[R("ffc")])
                    for j in range(NB):
                        fw.op("pe", lambda e, j=j: e.transpose(P[6][:, 0:6], fc[cur][:, j * 128:(j + 1) * 128], identf[0:6, 0:6]),
                              reads=rfc, writes=[RP[6]])
                        fw.op("dve", lambda e, j=j: e.tensor_scalar(nfk[:, j, :], P[6][:, 0:6], -1.0, 0.0, ALU.mult, ALU.add),
                              reads=[RP[6]], writes=[R("X", "nfk")])
                    fw.barrier()
                AC["q"] = [T(sl_, f"xq{i}", [128, S], BF16) for i in range(2)]
                AC["k"] = [T(sl_, f"xk{i}", [128, S], BF16) for i in range(2)]
                AC["v"] = [T(sl_, f"xv{i}", [128, NB, 128], BF16) for i in range(2)]
                AC["fqb"] = [T(sl_, f"fqb{i}", [128, S]) for i in range(2)]
                AC["x"] = [T(sl_, f"xx{i}", [128, 512]) for i in range(6)]
                AC["pt"] = [T(sl_, f"xp{i}", [128, 512], BF16) for i in range(6)]
                AC["rden"] = [T(sl_, f"rden{i}", [128, 512]) for i in range(2)]
                AC["ot"] = [T(sl_, f"xo{i}", [128, 512], BF16) for i in range(2)]
                AC["kb32"] = T(sl_, "kb32", [128, 16]); AC["kbar"] = T(sl_, "kbar", [128, 16], BF16)
                AC["gm"] = T(sl_, "gm", [128, 32, 16]); AC["m8"] = T(sl_, "m8", [128, 32, 8])
                AC["sel"] = T(sl_, "sel", [128, 32, 16]); AC["selb"] = T(sl_, "selb", [128, 32, 32], BF16)
                AC["selF"] = T(sl_, "selF", [128, 512])
                AC["selT"] = [T(sl_, f"selT{i}", [128, S], BF16) for i in range(2)]
                XS = os.environ.get("XSUB", "FM")
                hcnt = [0]
                if l + 1 < L and "w" in os.environ.get("PHASES", "wAHXC"):
                    fw.op("dve", lambda e: e.memset(gate_t[:], 0.0), writes=[R("gate", l)])
                    cast_weights(l + 1, gate=R("gate", l))
                for kind in ("fox", "moba"):
                    if {"fox": "F", "moba": "M"}[kind] not in XS:
                        continue
                    for h in range(6):
                        attn_head(l, kind, h, nfk, AC, hcnt[0])
                        hcnt[0] += 1
            fw.barrier()

        def attn_head(l, kind, h, nfk, AC, hc):
            cmaskb, qterm, kcol, indb, pastm, ownm = (AC[k_] for k_ in ("cmaskb", "qterm", "kcol", "indb", "pastm", "ownm"))
            fox = kind == "fox"
            qs, ks, vs = (fx_q, fx_k, fx_v) if fox else (mb_q, mb_k, mb_v)
            qn, kn, vn = ("bq", "bk", "bv") if fox else ("cq", "ck", "cv")
            mrow = (4 + h) * 128 if fox else (10 + h) * 128
            hb = hc % 2
            q, k, v, fqb = AC["q"][hb], AC["k"][hb], AC["v"][hb], AC["fqb"][hb]
            x, pt, rden, ot = AC["x"], AC["pt"], AC["rden"], AC["ot"]
            Rq, Rk, Rv, Rf = R("X", "q", hb), R("X", "k", hb), R("X", "v", hb), R("X", "fqb", hb)
            fw.dma(q[:], qs[h], reads=[R(qn, h, t) for t in range(NT)], writes=[Rq])
            fw.dma(k[:], ks[h], reads=[R(kn, h, t) for t in range(NT)], writes=[Rk])
            fw.dma(v[:], vs.rearrange("(j p) n -> p j n", p=128)[:, :, h * 128:(h + 1) * 128],
                   reads=[R(vn, tb, pc) for tb in range(NB) for pc in range(3)], writes=[Rv])
            if fox:
                fw.dma(fqb[:], fx_fc[h:h + 1, :].broadcast_to([128, S]), reads=[R("ffc")], writes=[Rf])
            else:
                kb32, kbar, gm, m8, sel, selb, selF = (AC[k_] for k_ in ("kb32", "kbar", "gm", "m8", "sel", "selb", "selF"))
                selT = AC["selT"][hb]
                RsT = R("M", "selT", hb)
                nblk = S // 256
                fw.op("dve", lambda e: e.memset(kb32[:], 0.0), writes=[R("M", "kb32")])
                fw.op("dve", lambda e: e.memset(selb[:], 0.0), writes=[R("M", "selb")])
                fw.op("dve", lambda e: e.tensor_reduce(kb32[:, 0:nblk], k[:].rearrange("p (n t) -> p n t", t=256), AX.X, ALU.add),
                      reads=[Rk], writes=[R("M", "kb32")])
                fw.op("act", lambda e: e.mul(kbar[:], kb32[:], 1.0 / 256), reads=[R("M", "kb32")], writes=[R("M", "kbar")])
                for qb in range(NB):
                    fw.op("pe", lambda e, qb=qb: e.matmul(P[6][:, qb * 16:(qb + 1) * 16], q[:, qb * 128:(qb + 1) * 128], kbar[:], start=True, stop=True),
                          reads=[Rq, R("M", "kbar")], writes=[RP[6]])
                g2 = gm[:].rearrange("p a b -> p (a b)")
                fw.op("dve", lambda e: e.tensor_tensor(g2[:, 0:NB * 16], P[6][:, 0:NB * 16], pastm[:].rearrange("p a b -> p (a b)")[:, 0:NB * 16], ALU.add),
                      reads=[RP[6]], writes=[R("M", "gm")])
                for qb in range(NB):
                    fw.op("dve", lambda e, qb=qb: e.max(m8[:, qb, :], gm[:, qb, :]), reads=[R("M", "gm")], writes=[R("M", "m8")])
                fw.op("dve", lambda e: e.tensor_tensor(sel[:, 0:NB, :], gm[:, 0:NB, :], m8[:, 0:NB, 2:3].broadcast_to([128, NB, 16]), ALU.is_ge),
                      reads=[R("M", "gm"), R("M", "m8")], writes=[R("M", "sel")])
                fw.op("dve", lambda e: e.tensor_scalar(sel[:, 0:NB, :], sel[:, 0:NB, :], SELB, -SELB, ALU.mult, ALU.add),
                      reads=[R("M", "sel")], writes=[R("M", "sel")])
                fw.op("dve", lambda e: e.tensor_tensor(selb[:, 0:NB, 0:16], sel[:, 0:NB, :], ownm[:, 0:NB, :], ALU.max),
                      reads=[R("M", "sel")], writes=[R("M", "selb")])
                for g4 in range(NB // 4):
                    for pa in range(4):
                        qb = g4 * 4 + pa
                        fw.op("pe", lambda e, qb=qb, pa=pa: e.matmul(P[6][0:32, pa * 128:(pa + 1) * 128], selb[:, qb, :], identb[:], start=True, stop=True),
                              reads=[R("M", "selb"), R("M", "selF")], writes=[RP[6]])
                    fw.op("dve", lambda e: e.tensor_copy(selF[:], P[6][:]), reads=[RP[6]], writes=[R("M", "selF")])
                    fw.op("dve", lambda e, g4=g4: e.tensor_copy(selT[0:32, g4 * 512:(g4 + 1) * 512], selF[0:32, :]),
                          reads=[R("M", "selF")], writes=[RsT])
            pairs = [(j, kb) for j in range(NT) for kb in range(4 * j + 4)]
            npair = len(pairs)
            LA = 3
            NSB = 4
            NXB = 6

            def isdiag(i):
                j, kb = pairs[i]
                return kb >= 4 * j

            def qk_main(i):
                j, kb = pairs[i]
                sb = i % NSB
                qsl = slice(j * 512, (j + 1) * 512)
                fw.op("pe", lambda e: e.matmul(P[sb][:], k[:, kb * 128:(kb + 1) * 128], q[:, qsl], start=True, stop=(fox and not isdiag(i))),
                      reads=[Rk, Rq], writes=[RP[sb]])

            def qk_sel(i):
                j, kb = pairs[i]
                sb = i % NSB
                qsl = slice(j * 512, (j + 1) * 512)
                fw.op("pe", lambda e: e.matmul(P[sb][:], indb[:, kb // 2, :], selT[0:32, qsl], start=False, stop=(not isdiag(i))),
                      reads=[RsT], writes=[RP[sb]])

            def qk_mask(i):
                j, kb = pairs[i]
                sb = i % NSB
                r = kb - 4 * j
                fw.op("pe", lambda e: e.matmul(P[sb][:], identb[:], cmaskb[:, r, :], start=False, stop=True), writes=[RP[sb]])

            def emit_soft(i):
                j, kb = pairs[i]
                sb = i % NSB
                xb = i % NXB
                qsl = slice(j * 512, (j + 1) * 512)
                if fox:
                    fw.op("dve", lambda e: e.tensor_tensor(x[xb][:], P[sb][:], fqb[:, qsl], ALU.add),
                          reads=[RP[sb], Rf], writes=[R("X", "x", xb)])
                    bias = nfk[:, kb, h:h + 1]
                else:
                    fw.op("dve", lambda e: e.tensor_tensor(x[xb][:], P[sb][:], qterm[:, h, :], ALU.add),
                          reads=[RP[sb]], writes=[R("X", "x", xb)])
                    delta = 4 * j - kb
                    bias = kcol[:, h, delta + 3:delta + 4]
                fw.op("act", lambda e: e.activation(pt[xb][:], x[xb][:], AF.Exp, bias=bias),
                      reads=[R("X", "x", xb), R("X", "nfk")], writes=[R("X", "pt", xb)])

            def pv(i):
                j, kb = pairs[i]
                xb = i % NXB
                nkb = 4 * j + 4
                po = 4 + (j % 2)
                fw.op("pe", lambda e: e.matmul(P[po][:], v[:, kb, :], pt[xb][:], start=(kb == 0), stop=(kb == nkb - 1)),
                      reads=[Rv, R("X", "pt", xb)], writes=[RP[po]])

            def den(i, it):
                j, kb = pairs[i]
                xb = i % NXB
                nkb = 4 * j + 4
                po = 4 + (j % 2)
                pd = 6
                fw.op("pe", lambda e: e.matmul(P[pd][:], ones1[:], pt[xb][:], start=(kb == 0), stop=(kb == nkb - 1)),
                      reads=[R("X", "pt", xb)], writes=[RP[pd]])
                if kb == nkb - 1:
                    oi = j % 2
                    qsl = slice(j * 512, (j + 1) * 512)
                    fw.op("act", lambda e: e.activation(rden[oi][:], P[pd][:], AF.Ln), reads=[RP[pd]], writes=[R("X", "rden", oi)])
                    fw.op("act", lambda e: e.activation(rden[oi][:], rden[oi][:], AF.Exp, scale=-1.0),
                          reads=[R("X", "rden", oi)], writes=[R("X", "rden", oi)])

                    def fin():
                        fw.op("dve", lambda e: e.tensor_tensor(ot[oi][:], P[po][:], rden[oi][:], ALU.mult),
                              reads=[RP[po], R("X", "rden", oi)], writes=[R("X", "ot", oi)])
                        fw.dma(mixT[mrow:mrow + 128, qsl], ot[oi][:], reads=[R("X", "ot", oi)], writes=[R("mix", mrow // 128, j)])
                    fins.append((it + 3, fin))

            fins = []
            for i in range(npair + LA):
                ii = i - LA
                if ii >= 0:
                    emit_soft(ii)
                while fins and fins[0][0] <= i:
                    fins.pop(0)[1]()
                if i < npair:
                    qk_main(i)
                if ii >= 0:
                    pv(ii)
                if i < npair and not fox:
                    qk_sel(i)
                if ii >= 0:
                    den(ii, i)
                if i < npair and isdiag(i):
                    qk_mask(i)
            while fins:
                fins.pop(0)[1]()

        def phase_C(l, last):
            hsrc = xT if l == 0 else hbuf
            hdst = outT if last else hbuf
            hs3 = hsrc.rearrange("(c p) s -> p c s", p=128)
            hd3 = hdst.rearrange("(c p) s -> p c s", p=128)
            mx3 = mixT.rearrange("(c p) s -> p c s", p=128)
            with ExitStack() as st_:
                ht = T(st_, "ht", [128, KC, 512])
                ct = T(st_, "ct", [128, KC, 512], BF16)
                mt = ct
                st = dict(sq0=T(st_, "csq0", [128, 512], BF16), sq1=T(st_, "csq1", [128, 512], BF16),
                          rs=T(st_, "crs", [128, 512]), rstd=T(st_, "crstd", [128, 512]))
                hid = T(st_, "hid", [128, FC, 512], BF16)
                wp = [T(st_, f"cwp{i}", [128, KC, 256], BF16) for i in range(3)]
                wd = [T(st_, f"cwd{i}", [128, FC, 128], BF16) for i in range(2)]
                wq = T(st_, "cwq", [128, 2, D], BF16)
                hg = [T(st_, f"hg{i}", [128, 514]) for i in range(2)]
                halo = T(st_, "halo", [128, FC, 2])
                yy = [T(st_, f"yy{i}", [128, 512]) for i in range(2)]
                gl = [T(st_, f"gl{i}", [128, 512]) for i in range(2)]
                pf = T(st_, "pf", [128, 2, 512]); pb16 = T(st_, "pb16", [128, 2, 512], BF16)
                sg = [T(st_, f"sg{i}", [128, 512]) for i in range(2)]
                cnt = dict(wp=0, wd=0, p=0, e=0)
                fw.op("dve", lambda e: e.memset(halo[:], 0.0), writes=[R("C", "halo")])
                fw.dma(wq[:], Wb["w_pp"][l].rearrange("(c p) n -> p c n", p=128), reads=wres("w_pp", l), writes=[R("C", "wq")])

                def load_panel(wname, col0):
                    i = cnt["wp"] % 3
                    cnt["wp"] += 1
                    src = Wb[wname][l].rearrange("(c p) n -> p c n", p=128)[:, :, col0:col0 + 256]
                    fw.dma(wp[i][:], src, reads=wres(wname, l), writes=[R("C", "wp", i)])
                    return i

                def mm16(pb, wi, g, rhs, rres):
                    for kc in range(KC):
                        fw.op("pe", lambda e, kc=kc: e.matmul(P[pb][:], wp[wi][:, kc, g * 128:(g + 1) * 128], rhs[:, kc, :],
                                                              start=(kc == 0), stop=(kc == KC - 1)),
                              reads=[R("C", "wp", wi), R("C", "ct", kc)], writes=[RP[pb]])

                for t in range(NT):
                    tsl = slice(t * 512, (t + 1) * 512)
                    fw.dma(ht[:], hs3[:, :, tsl], reads=[R("h", t)], writes=[R("C", "ht")])
                    fw.dma(mt[:], mx3[:, :, tsl], reads=[R("mix", c, t) for c in range(KC)], writes=[R("C", "ct", c) for c in range(KC)])
                    fw.dma(pf[:], pT[l].rearrange("(c p) s -> p c s", p=128)[:, :, tsl], writes=[R("C", "pf")])
                    fw.op("pool", lambda e: e.tensor_copy(pb16[:], pf[:]), reads=[R("C", "pf")], writes=[R("C", "pb16")])
                    for pc in range(D // 256):
                        wi = load_panel("w_out", pc * 256)
                        for g in range(2):
                            c = pc * 2 + g
                            pb = cnt["p"] % 2
                            cnt["p"] += 1
                            mm16(pb, wi, g, mt, R("C", "ct"))
                            fw.op("dve", lambda e, c=c, pb=pb: e.tensor_tensor(ht[:, c, :], ht[:, c, :], P[pb][:], ALU.add),
                                  reads=[RP[pb]], writes=[R("C", "ht")])
                    rmsnorm_tile(st, ht, gF, l, lambda c: ct[:, c, :], R("C", "ht"), (lambda c: R("C", "ct", c)), "Cn")
                    for pc in range(DFF // 256):
                        wg = load_panel("w_gate", pc * 256)
                        wu = load_panel("w_up", pc * 256)
                        for g in range(2):
                            f = pc * 2 + g
                            ei = cnt["e"] % 2
                            cnt["e"] += 1
                            pg_, pu_ = (0, 1) if ei == 0 else (4, 5)
                            mm16(pg_, wg, g, ct, R("C", "ct"))
                            fw.op("act", lambda e, ei=ei, pg_=pg_: e.copy(hg[ei][:, 2:514], P[pg_][:]), reads=[RP[pg_]], writes=[R("C", "hg", ei)])
                            mm16(pu_, wu, g, ct, R("C", "ct"))
                            fw.op("pool", lambda e, ei=ei, f=f: e.tensor_copy(hg[ei][:, 0:2], halo[:, f, :]),
                                  reads=[R("C", "halo")], writes=[R("C", "hg", ei)])
                            fw.op("act", lambda e, ei=ei, f=f: e.activation(yy[ei][:], hg[ei][:, 2:514], AF.Identity,
                                                                            bias=convb[:, l, f:f + 1], scale=convw[:, l, 2, f:f + 1]),
                                  reads=[R("C", "hg", ei)], writes=[R("C", "yy", ei)])
                            fw.op("dve", lambda e, ei=ei, f=f: e.scalar_tensor_tensor(yy[ei][:], hg[ei][:, 1:513], convw[:, l, 1, f:f + 1], yy[ei][:], ALU.mult, ALU.add),
                                  reads=[R("C", "hg", ei), R("C", "yy", ei)], writes=[R("C", "yy", ei)])
                            fw.op("dve", lambda e, ei=ei, f=f: e.scalar_tensor_tensor(yy[ei][:], hg[ei][:, 0:512], convw[:, l, 0, f:f + 1], yy[ei][:], ALU.mult, ALU.add),
                                  reads=[R("C", "hg", ei), R("C", "yy", ei)], writes=[R("C", "yy", ei)])
                            fw.op("pool", lambda e, ei=ei, f=f: e.tensor_copy(halo[:, f, :], hg[ei][:, 512:514]),
                                  reads=[R("C", "hg", ei)], writes=[R("C", "halo")])
                            fw.op("act", lambda e, ei=ei: e.activation(gl[ei][:], yy[ei][:], AF.Gelu_apprx_tanh),
                                  reads=[R("C", "yy", ei)], writes=[R("C", "gl", ei)])
                            fw.op("dve", lambda e, ei=ei, f=f, pu_=pu_: e.tensor_tensor(hid[:, f, :], gl[ei][:], P[pu_][:], ALU.mult),
                                  reads=[R("C", "gl", ei), RP[pu_]], writes=[R("C", "hid")])
                    for c in range(KC):
                        di = cnt["wd"] % 2
                        cnt["wd"] += 1
                        fw.dma(wd[di][:], Wb["w_down"][l].rearrange("(f p) n -> p f n", p=128)[:, :, c * 128:(c + 1) * 128],
                               reads=wres("w_down", l), writes=[R("C", "wd", di)])
                        pb = 2 + c % 2
                        for f in range(FC):
                            fw.op("pe", lambda e, f=f, di=di, pb=pb: e.matmul(P[pb][:], wd[di][:, f, :], hid[:, f, :], start=(f == 0), stop=(f == FC - 1)),
                                  reads=[R("C", "wd", di), R("C", "hid")], writes=[RP[pb]])
                        fw.op("dve", lambda e, c=c, pb=pb: e.tensor_tensor(ht[:, c, :], ht[:, c, :], P[pb][:], ALU.add),
                              reads=[RP[pb]], writes=[R("C", "ht")])
                    rmsnorm_tile(st, ht, gP, l, lambda c: ct[:, c, :], R("C", "ht"), (lambda c: R("C", "ct", c)), "Cn")
                    for pc in range(D // 256):
                        wi = load_panel("w_pg", pc * 256)
                        for g in range(2):
                            c = pc * 2 + g
                            pb = cnt["p"] % 2
                            cnt["p"] += 1
                            si = c % 2
                            mm16(pb, wi, g, ct, R("C", "ct"))
                            fw.op("act", lambda e, pb=pb, si=si: e.activation(sg[si][:], P[pb][:], AF.Sigmoid), reads=[RP[pb]], writes=[R("C", "sg", si)])
                            p2 = 4 + c % 2
                            for kc in range(2):
                                fw.op("pe", lambda e, kc=kc, c=c, p2=p2: e.matmul(P[p2][:], wq[:, kc, c * 128:(c + 1) * 128], pb16[:, kc, :], start=(kc == 0), stop=(kc == 1)),
                                      reads=[R("C", "wq"), R("C", "pb16")], writes=[RP[p2]])
                            fw.op("dve", lambda e, si=si, p2=p2: e.tensor_tensor(sg[si][:], sg[si][:], P[p2][:], ALU.mult),
                                  reads=[R("C", "sg", si), RP[p2]], writes=[R("C", "sg", si)])
                            fw.op("dve", lambda e, c=c, si=si: e.tensor_tensor(ht[:, c, :], ht[:, c, :], sg[si][:], ALU.add),
                                  reads=[R("C", "sg", si)], writes=[R("C", "ht")])
                    fw.dma(hd3[:, :, tsl], ht[:], reads=[R("C", "ht")], writes=[R("h", t)])
            fw.barrier()

        PH = os.environ.get("PHASES", "wAHXC")
        if "w" in PH:
            cast_weights(0)
        for l in range(L):
            if "A" in PH:
                phase_A(l)
            if "H" in PH:
                phase_hgrn(l)
            if "X" in PH:
                phase_attn(l)
            if "C" in PH:
                phase_C(l, l == L - 1)
        fw.barrier(all_dma=True)
        print("instructions:", fw.ninst, {k: v for k, v in fw.ccount.items()})
    return nc


def host_consts():
    slopes = np.exp2(-8.0 * np.arange(1, 7, dtype=np.float32) / 6).astype(np.float32)
    p = np.arange(128)
    q = np.arange(512)
    cmask = np.zeros((128, 4, 512), np.float32)
    for r in range(4):
        cmask[:, r, :] = np.where((r * 128 + p)[:, None] <= q[None, :], 0.0, NEGM)
    hmask = (np.arange(64)[:, None] <= np.arange(64)[None, :]).astype(np.float32)
    qterm = np.broadcast_to((-slopes[:, None] * q[None, :].astype(np.float32))[None], (128, 6, 512)).astype(np.float32).copy()
    delta = np.arange(-3, 37).astype(np.float32)
    kcol = (slopes[None, :, None] * (p[:, None, None].astype(np.float32) - 128.0 * delta[None, None, :])).astype(np.float32)
    ind = np.zeros((32, 16, 128), np.float32)
    for n in range(16):
        ind[n, n, :] = 1.0
    own = (np.arange(32) // 2)[:, None]
    n = np.arange(16)[None, :]
    pastm = np.broadcast_to(np.where(n < own, 0.0, NEGM)[None], (128, 32, 16)).astype(np.float32).copy()
    ownm = np.broadcast_to(np.where(n < own, -SELB, 0.0)[None], (128, 32, 16)).astype(np.float32).copy()
    return dict(ident=np.eye(128, dtype=np.float32), cmask=cmask, hmask=hmask, qterm=qterm, kcol=kcol, ind=ind,
                pastm=pastm, ownm=ownm)


def pm(v, L):
    v = np.asarray(v, np.float32)
    return np.ascontiguousarray(v.reshape(L, -1, 128).transpose(2, 0, 1))


def make_in_maps(inp, S, L, nb):
    f32 = lambda a: np.ascontiguousarray(np.asarray(a, np.float32))
    shared = dict(
        w_in=f32(inp["w_in"][:L]), w_out=f32(inp["w_out"][:L]), w_gate=f32(inp["w_gate"][:L]), w_up=f32(inp["w_up"][:L]),
        w_down=f32(inp["w_down"][:L]), w_pg=f32(inp["w_ple_gate"][:L]), w_pp=f32(inp["w_ple_proj"][:L]),
        gA=pm(inp["attn_norm"][:L], L), gF=pm(inp["ffn_norm"][:L], L), gP=pm(inp["ple_norm"][:L], L),
        convw=np.ascontiguousarray(np.asarray(inp["conv_w"][:L], np.float32).reshape(L, 3, FC, 128).transpose(3, 0, 1, 2)),
        convb=pm(inp["conv_b"][:L], L), lbl=pm(inp["lb_logits"][:L], L),
        hv=np.ascontiguousarray(np.stack([np.asarray(inp[k][:L], np.float32).T for k in
                                          ("hgrn_onorm", "fox_qnorm", "fox_knorm", "moba_qnorm", "moba_knorm")], axis=1)),
        fbf=np.ascontiguousarray(np.asarray(inp["fox_bf"][:L], np.float32).T),
        **host_consts())
    maps = []
    for b in range(nb):
        m = dict(shared)
        m["xT"] = np.ascontiguousarray(np.asarray(inp["x"][b, :S], np.float32).T)
        m["pT"] = np.ascontiguousarray(np.asarray(inp["p"][:L, b, :S], np.float32).transpose(0, 2, 1))
        maps.append(m)
    return maps


_NC_CACHE = {}


def kernel(**inputs):
    S, L, B = 4096, 4, 4
    if (S, L) not in _NC_CACHE:
        _NC_CACHE[(S, L)] = build(S, L)
    nc = _NC_CACHE[(S, L)]
    maps = make_in_maps(inputs, S, L, B)
    res = run_bass_kernel_spmd(nc, maps, core_ids=list(range(B)))
    out = np.stack([np.asarray(r["outT"], np.float32).T for r in res.results], axis=0)
    return np.ascontiguousarray(out)
```

```python
import os
from contextlib import ExitStack
import numpy as np
import concourse.bass as bass
import concourse.mybir as mybir
from concourse.bass_utils import run_bass_kernel_spmd

F32 = mybir.dt.float32
BF16 = mybir.dt.bfloat16
AF = mybir.ActivationFunctionType
ALU = mybir.AluOpType
AX = mybir.AxisListType

D = 2048
KC = 16
DFF = 5632
FC = 44
PLE = 256
INC = 6662
EPS = 1e-6
NEGM = -1e30
SELB = 30000.0
COL = dict(aq=0, af=512, ai=1024, ag=1536, bq=2048, bk=2816, bv=3584, bf=4352, cq=4358, ck=5126, cv=5894)
NDSEM = 40


class Res:
    __slots__ = ("name", "w", "r")

    def __init__(self, name):
        self.name = name
        self.w = None
        self.r = {}


class FW:
    ENGS = ("pe", "act", "dve", "pool", "sp")

    def __init__(self, nc, es):
        self.nc = nc
        self.csem = {e: es.enter_context(nc.semaphore("c_" + e)) for e in ("pe", "act", "dve", "pool")}
        self.ccount = {e: 0 for e in self.csem}
        self.dsem = [es.enter_context(nc.semaphore(f"d{i}")) for i in range(NDSEM)]
        self.dcount = [0] * NDSEM
        self.dnext = {"pool": 0, "sp": 8}
        self.drange = {"pool": (0, 3), "sp": (8, NDSEM)}
        self.seen = {e: {} for e in self.ENGS}
        self.res = {}
        self.engobj = {"pe": nc.tensor, "act": nc.scalar, "dve": nc.vector, "pool": nc.gpsimd, "sp": nc.sync}
        self.ninst = 0

    def R(self, *key):
        r = self.res.get(key)
        if r is None:
            r = self.res[key] = Res(key)
        return r

    def _wait(self, eng, key, n):
        if key == eng and eng == "pe":
            return
        if self.seen[eng].get(key, 0) >= n:
            return
        self.seen[eng][key] = n
        sem = self.csem[key] if isinstance(key, str) else self.dsem[key]
        self.engobj[eng].wait_ge(sem, n)
        self.ninst += 1

    def _deps(self, eng, reads, writes):
        for r in reads:
            if r.w is not None:
                self._wait(eng, *r.w)
        for w in writes:
            if w.w is not None:
                self._wait(eng, *w.w)
            for k, n in w.r.items():
                self._wait(eng, k, n)

    def _commit(self, tok, reads, writes):
        k, n = tok
        for r in reads:
            if r.r.get(k, 0) < n:
                r.r[k] = n
        for w in writes:
            w.w = tok
            w.r = {}

    def op(self, eng, fn, reads=(), writes=()):
        self._deps(eng, reads, writes)
        self.ccount[eng] += 1
        fn(self.engobj[eng]).then_inc(self.csem[eng], 1)
        self.ninst += 1
        self._commit((eng, self.ccount[eng]), reads, writes)

    def dma(self, out, in_, reads=(), writes=(), q="sp", **kw):
        self._deps(q, reads, writes)
        i = self.dnext[q]
        lo, hi = self.drange[q]
        self.dnext[q] = lo + (i + 1 - lo) % (hi - lo)
        if self.dcount[i] > 0:
            self._wait(q, i, self.dcount[i])
        self.dcount[i] += 16
        self.engobj[q].dma_start(out=out, in_=in_, **kw).then_inc(self.dsem[i], 16)
        self.ninst += 1
        self._commit((i, self.dcount[i]), reads, writes)

    def barrier(self, all_dma=False):
        lo = 0 if all_dma else self.drange["pool"][1]
        for e in self.ENGS:
            for e2 in self.csem:
                if e2 != e and self.ccount[e2] > 0:
                    self._wait(e, e2, self.ccount[e2])
            for i in range(lo, NDSEM):
                if self.dcount[i] > 0:
                    self._wait(e, i, self.dcount[i])


def build(S, L, dbg=()):
    NT = S // 512
    NB = S // 128
    NCH = S // 64
    TA = min(S, 2048)
    nc = bass.Bass("TRN2", target_bir_lowering=False)

    def ext_in(name, shape, dt=F32):
        return nc.dram_tensor(name, list(shape), dt, kind="ExternalInput").ap()

    def scratch(name, shape, dt):
        kind = "ExternalOutput" if name in dbg else "Internal"
        return nc.dram_tensor(name, list(shape), dt, kind=kind).ap()

    xT = ext_in("xT", [D, S])
    pT = ext_in("pT", [L, PLE, S])
    W32 = dict(
        w_in=ext_in("w_in", [L, D, INC]), w_out=ext_in("w_out", [L, D, D]),
        w_gate=ext_in("w_gate", [L, D, DFF]), w_up=ext_in("w_up", [L, D, DFF]),
        w_down=ext_in("w_down", [L, DFF, D]), w_pg=ext_in("w_pg", [L, D, D]),
        w_pp=ext_in("w_pp", [L, PLE, D]))
    gA_d = ext_in("gA", [128, L, KC]); gF_d = ext_in("gF", [128, L, KC]); gP_d = ext_in("gP", [128, L, KC])
    convw_d = ext_in("convw", [128, L, 3, FC]); convb_d = ext_in("convb", [128, L, FC])
    lbl_d = ext_in("lbl", [128, L, 4])
    hv_d = ext_in("hv", [128, 5, L])
    fbf_d = ext_in("fbf", [6, L])
    ident_d = ext_in("ident", [128, 128])
    cmask_d = ext_in("cmask", [128, 4, 512])
    hmask_d = ext_in("hmask", [64, 64])
    qterm_d = ext_in("qterm", [128, 6, 512])
    kcol_d = ext_in("kcol", [128, 6, 40])
    ind_d = ext_in("ind", [32, 16, 128])
    pastm_d = ext_in("pastm", [128, 32, 16])
    ownm_d = ext_in("ownm", [128, 32, 16])
    outT = nc.dram_tensor("outT", [D, S], F32, kind="ExternalOutput").ap()

    hbuf = scratch("hbuf", [D, S], F32)
    Wb = {k: scratch("b_" + k, v.shape, BF16) for k, v in W32.items()}
    hg_q = scratch("hg_q", [4, 128, S], BF16); hg_k = scratch("hg_k", [4, 128, S], BF16)
    hg_g = scratch("hg_g", [4, 128, S], BF16); hg_lf = scratch("hg_lf", [4, 128, S], F32)
    hg_v = scratch("hg_v", [S, 512], BF16)
    fx_q = scratch("fx_q", [6, 128, S], BF16); fx_k = scratch("fx_k", [6, 128, S], BF16)
    fx_v = scratch("fx_v", [S, 768], BF16); fx_f = scratch("fx_f", [6, S], F32); fx_fc = scratch("fx_fc", [6, S], F32)
    mb_q = scratch("mb_q", [6, 128, S], BF16); mb_k = scratch("mb_k", [6, 128, S], BF16)
    mb_v = scratch("mb_v", [S, 768], BF16)
    mixT = scratch("mixT", [D, S], BF16)

    es = ExitStack()
    with es:
        fw = FW(nc, es)
        R = fw.R

        uid = [0]

        def T(st, name, shape, dt=F32):
            uid[0] += 1
            return st.enter_context(nc.sbuf_tensor(f"s{uid[0]}_{name}", list(shape), dt))

        P = [es.enter_context(nc.psum_tensor(f"P{i}", [128, 512], F32)) for i in range(7)]
        PB = es.enter_context(nc.psum_tensor("PB", [128, 1024], BF16))
        RP = [R("P", i) for i in range(7)]
        RPB = [R("PB", 0), R("PB", 1)]

        cst = es
        gA = T(cst, "gA", [128, L, KC]); gF = T(cst, "gF", [128, L, KC]); gP = T(cst, "gP", [128, L, KC])
        convw = T(cst, "convw", [128, L, 3, FC]); convb = T(cst, "convb", [128, L, FC])
        lbl = T(cst, "lbl", [128, L, 4]); hv = T(cst, "hv", [128, 5, L]); fbf = T(cst, "fbf", [6, L])
        identf = T(cst, "identf", [128, 128]); identb = T(cst, "identb", [128, 128], BF16)
        hmask = T(cst, "hmask", [64, 64])
        gate_t = T(cst, "gate_t", [128, 8])
        indb = T(cst, "indb", [32, 16, 128], BF16)
        onesD = T(cst, "onesD", [128, 128], BF16); onesH = T(cst, "onesH", [128, 128], BF16)
        ones1 = T(cst, "ones1", [128, 128], BF16)
        lb = T(cst, "lb", [128, L, 4]); oml = T(cst, "oml", [128, L, 4]); noml = T(cst, "noml", [128, L, 4])
        lbe = T(cst, "lbe", [128, L, 4]); lbs = T(cst, "lbs", [128, 4])
        gqs = T(cst, "gqs", [128, 2, L])
        rc = R("consts")
        for dst, src in ((gA, gA_d), (gF, gF_d), (gP, gP_d), (convw, convw_d), (convb, convb_d), (lbl, lbl_d),
                         (hv, hv_d), (fbf, fbf_d), (identf, ident_d), (hmask, hmask_d)):
            fw.dma(dst[:], src, writes=[R("c", len(fw.res))])
        fw.dma(indb[:], ind_d, writes=[R("c", len(fw.res))], q="pool")
        fw.barrier(all_dma=True)
        fw.op("dve", lambda e: e.tensor_copy(identb[:], identf[:]), writes=[rc])
        fw.op("pool", lambda e: e.memset(onesD[:], 1.0 / D), writes=[rc])
        fw.op("pool", lambda e: e.memset(onesH[:], 1.0 / 128), writes=[rc])
        fw.op("pool", lambda e: e.memset(ones1[:], 1.0), writes=[rc])
        fw.op("act", lambda e: e.activation(lbe[:], lbl[:], AF.Exp), writes=[rc])
        fw.barrier()
        fw.op("dve", lambda e: e.tensor_copy(lbs[:], lbe[:, 0, :]), writes=[rc])
        for l in range(1, L):
            fw.op("dve", lambda e, l=l: e.tensor_tensor(lbs[:], lbs[:], lbe[:, l, :], ALU.add), writes=[rc])
        fw.op("dve", lambda e: e.reciprocal(lbs[:], lbs[:]), writes=[rc])
        for l in range(L):
            fw.op("dve", lambda e, l=l: e.tensor_tensor(lbe[:, l, :], lbe[:, l, :], lbs[:], ALU.mult), writes=[rc])
        fw.op("dve", lambda e: e.memset(lb[:, 0, :], 0.0), writes=[rc])
        for l in range(1, L):
            fw.op("dve", lambda e, l=l: e.tensor_tensor(lb[:, l, :], lb[:, l - 1, :], lbe[:, l, :], ALU.add), writes=[rc])
        fw.op("dve", lambda e: e.tensor_scalar(oml[:], lb[:], -1.0, 1.0, ALU.mult, ALU.add), writes=[rc])
        fw.op("dve", lambda e: e.tensor_scalar(noml[:], lb[:], 1.0, -1.0, ALU.mult, ALU.add), writes=[rc])
        fw.op("dve", lambda e: e.tensor_scalar(gqs[:, 0, :], hv[:, 1, :], 128 ** -0.5, 0.0, ALU.mult, ALU.add), writes=[rc])
        fw.op("dve", lambda e: e.tensor_scalar(gqs[:, 1, :], hv[:, 3, :], 128 ** -0.5, 0.0, ALU.mult, ALU.add), writes=[rc])
        fw.barrier()

        A_F = [("aq", 4), ("af", 4), ("ag", 4), ("bq", 6), ("bk", 6), ("cq", 6), ("ck", 6)]
        A_T = [("ai", 512), ("bv", 768), ("cv", 768)]
        A_ORDER = [COL[nm] + pc * 256 for nm, nh in A_F for pc in range(nh // 2)] + \
                  [COL[nm] + pc * 256 for nm, nc_ in A_T for pc in range(nc_ // 256)]

        def cast_weights(l, gate=None):
            src = W32["w_in"]
            g = [gate] if gate is not None else []
            for col0 in A_ORDER[:6] + [COL["bf"]] + A_ORDER[6:]:
                ncols = 6 if col0 == COL["bf"] else 256
                fw.dma(Wb["w_in"][l, :, col0:col0 + ncols], src[l, :, col0:col0 + ncols],
                       reads=g, writes=[R("wA", l, col0)], q="pool")
            for k, src in W32.items():
                if k == "w_in":
                    continue
                rows = src.shape[1]
                for rb in range(rows // 64):
                    fw.dma(Wb[k][l, rb * 64:(rb + 1) * 64, :], src[l, rb * 64:(rb + 1) * 64, :],
                           writes=[R("w", k, l, rb)], q="pool")

        def wres(k, l):
            return [R("w", k, l, rb) for rb in range(W32[k].shape[1] // 64)]

        def rmsnorm_tile(st, hin, gain, l, out_fn, rin, rout, tag):
            sq = [st[f"sq{i}"] for i in range(2)]
            for c in range(KC):
                b = c % 2
                fw.op("act", lambda e, c=c, b=b: e.activation(sq[b][:], hin[:, c, :], AF.Square),
                      reads=[rin], writes=[R(tag, "sq", b)])
                fw.op("pe", lambda e, c=c, b=b: e.matmul(P[6][:], onesD[:], sq[b][:], start=(c == 0), stop=(c == KC - 1)),
                      reads=[R(tag, "sq", b)], writes=[RP[6]])
            rs, rstd = st["rs"], st["rstd"]
            fw.op("act", lambda e: e.activation(rs[:], P[6][:], AF.Ln, bias=EPS), reads=[RP[6]], writes=[R(tag, "rs")])
            fw.op("act", lambda e: e.activation(rstd[:], rs[:], AF.Exp, scale=-0.5), reads=[R(tag, "rs")], writes=[R(tag, "rstd")])
            for c in range(KC):
                fw.op("dve", lambda e, c=c: e.scalar_tensor_tensor(out_fn(c), hin[:, c, :], gain[:, l, c:c + 1], rstd[:],
                                                                     ALU.mult, ALU.mult),
                      reads=[rin, R(tag, "rstd")], writes=[rout(c)])

        def phase_A(l):
            hsrc = xT if l == 0 else hbuf
            hv3 = hsrc.rearrange("(c p) s -> p c s", p=128)
            with ExitStack() as st_:
                aT = T(st_, "aT", [128, KC, TA], BF16)
                hins = [T(st_, f"hin{i}", [128, KC, 512]) for i in range(2)]
                st = dict(sq0=T(st_, "sq0", [128, 512], BF16), sq1=T(st_, "sq1", [128, 512], BF16),
                          rs=T(st_, "rs", [128, 512]), rstd=T(st_, "rstd", [128, 512]))
                NWP = 4
                wp = [T(st_, f"wp{i}", [128, KC, 256], BF16) for i in range(NWP)]
                wf = T(st_, "wf", [128, KC, 6], BF16)
                ob = [T(st_, f"ob{i}", [128, 512], BF16) for i in range(3)]
                of = [T(st_, f"of{i}", [128, 512]) for i in range(2)]
                s1 = [T(st_, f"s1{i}", [128, 512]) for i in range(2)]
                s2 = [T(st_, f"s2{i}", [128, 512]) for i in range(2)]
                qsq = [T(st_, f"qsq{i}", [128, 512], BF16) for i in range(2)]
                qrs = [T(st_, f"qrs{i}", [128, 512]) for i in range(2)]
                vb = [T(st_, f"vb{i}", [128, 256], BF16) for i in range(2)]
                cnt = dict(p=0, ob=0, of=0, q=0, wp=0, vb=0)

                pending = []
                seq = A_ORDER * (S // TA)
                issued = [0]

                def load_panel(col0):
                    n = cnt["wp"]
                    cnt["wp"] += 1
                    assert seq[n] == col0
                    while issued[0] < min(len(seq), n + NWP - 1):
                        m = issued[0]
                        issued[0] += 1
                        src = Wb["w_in"][l].rearrange("(c p) n -> p c n", p=128)[:, :, seq[m]:seq[m] + 256]
                        fw.dma(wp[m % NWP][:], src, reads=[R("wA", l, seq[m])], writes=[R("A", "wp", m % NWP)])
                    return n % NWP

                F_SEGS = [("aq", 4, "silu", hg_q), ("af", 4, "hf", None), ("ag", 4, "silu", hg_g),
                          ("bq", 6, "nq", fx_q), ("bk", 6, "nk", fx_k), ("cq", 6, "mq", mb_q), ("ck", 6, "mk", mb_k)]
                for half in range(S // TA):
                    nsub = TA // 512
                    for sub in range(nsub):
                        t = half * nsub + sub
                        hin = hins[t % 2]
                        if sub == 0:
                            fw.dma(hin[:], hv3[:, :, t * 512:(t + 1) * 512], reads=[R("h", t)], writes=[R("A", "hin", t % 2)])
                        if sub + 1 < nsub:
                            fw.dma(hins[(t + 1) % 2][:], hv3[:, :, (t + 1) * 512:(t + 2) * 512], reads=[R("h", t + 1)],
                                   writes=[R("A", "hin", (t + 1) % 2)])
                        rmsnorm_tile(st, hin, gA, l, lambda c, sub=sub: aT[:, c, sub * 512:(sub + 1) * 512],
                                     R("A", "hin", t % 2), (lambda c, sub=sub: R("A", "aT", sub, c)), "An")
                    for (nm, nh, epi, dst) in F_SEGS:
                        for pc in range(nh // 2):
                            wi = load_panel(COL[nm] + pc * 256)
                            for sub in range(nsub):
                                t = half * nsub + sub
                                tsl = slice(t * 512, (t + 1) * 512)
                                for g in range(2):
                                    head = pc * 2 + g
                                    pb = (0, 1, 4, 5)[cnt["p"] % 4]
                                    cnt["p"] += 1
                                    for kc in range(KC):
                                        fw.op("pe", lambda e, kc=kc, g=g, wi=wi, pb=pb, sub=sub: e.matmul(
                                            P[pb][:], wp[wi][:, kc, g * 128:(g + 1) * 128], aT[:, kc, sub * 512:(sub + 1) * 512],
                                            start=(kc == 0), stop=(kc == KC - 1)),
                                            reads=[R("A", "wp", wi), R("A", "aT", sub, kc)], writes=[RP[pb]])
                                    while pending:
                                        pending.pop(0)()
                                    if epi == "silu":
                                        oi = cnt["ob"] % 3
                                        cnt["ob"] += 1
                                        fw.op("act", lambda e, pb=pb, oi=oi: e.activation(ob[oi][:], P[pb][:], AF.Silu),
                                              reads=[RP[pb]], writes=[R("A", "ob", oi)])
                                        fw.dma(dst[head, :, tsl], ob[oi][:], reads=[R("A", "ob", oi)], writes=[R(nm, head, t)])
                                    elif epi == "hf":
                                        si = cnt["of"] % 2
                                        cnt["of"] += 1
                                        oi = cnt["ob"] % 3
                                        cnt["ob"] += 1
                                        fw.op("act", lambda e, pb=pb, si=si: e.activation(s1[si][:], P[pb][:], AF.Sigmoid),
                                              reads=[RP[pb]], writes=[R("A", "s1", si)])
                                        fw.op("dve", lambda e, si=si, oi=oi, head=head: e.tensor_scalar(
                                            ob[oi][:], s1[si][:], noml[:, l, head:head + 1], oml[:, l, head:head + 1], ALU.mult, ALU.add),
                                            reads=[R("A", "s1", si)], writes=[R("A", "ob", oi)])
                                        fw.dma(hg_k[head, :, tsl], ob[oi][:], reads=[R("A", "ob", oi)], writes=[R("hk", head, t)])
                                        fw.op("dve", lambda e, si=si, head=head: e.tensor_scalar(
                                            s2[si][:], s1[si][:], oml[:, l, head:head + 1], lb[:, l, head:head + 1], ALU.mult, ALU.add),
                                            reads=[R("A", "s1", si)], writes=[R("A", "s2", si)])
                                        fw.op("act", lambda e, si=si: e.activation(of[si][:], s2[si][:], AF.Ln),
                                              reads=[R("A", "s2", si)], writes=[R("A", "of", si)])
                                        fw.dma(hg_lf[head, :, tsl], of[si][:], reads=[R("A", "of", si)], writes=[R("hlf", head, t)])
                                    else:
                                        gain = {"nq": gqs[:, 0, l:l + 1], "nk": hv[:, 2, l:l + 1],
                                                "mq": gqs[:, 1, l:l + 1], "mk": hv[:, 4, l:l + 1]}[epi]
                                        qi = cnt["q"] % 2
                                        cnt["q"] += 1
                                        oi = cnt["ob"] % 3
                                        cnt["ob"] += 1
                                        fw.op("act", lambda e, pb=pb, qi=qi: e.activation(qsq[qi][:], P[pb][:], AF.Square),
                                              reads=[RP[pb]], writes=[R("A", "qsq", qi)])
                                        def tail(pb=pb, qi=qi, oi=oi, gain=gain, head=head, tsl=tsl, t=t, dst=dst, nm=nm):
                                            fw.op("pe", lambda e: e.matmul(P[2 + qi][:], onesH[:], qsq[qi][:], start=True, stop=True),
                                                  reads=[R("A", "qsq", qi)], writes=[RP[2 + qi]])
                                            fw.op("act", lambda e: e.activation(qrs[qi][:], P[2 + qi][:], AF.Ln, bias=EPS),
                                                  reads=[RP[2 + qi]], writes=[R("A", "qrs", qi)])
                                            fw.op("act", lambda e: e.activation(qrs[qi][:], qrs[qi][:], AF.Exp, scale=-0.5),
                                                  reads=[R("A", "qrs", qi)], writes=[R("A", "qrs", qi)])
                                            fw.op("dve", lambda e: e.scalar_tensor_tensor(
                                                ob[oi][:], P[pb][:], gain, qrs[qi][:], ALU.mult, ALU.mult),
                                                reads=[RP[pb], R("A", "qrs", qi)], writes=[R("A", "ob", oi)])
                                            fw.dma(dst[head, :, tsl], ob[oi][:], reads=[R("A", "ob", oi)], writes=[R(nm, head, t)])
                                        pending.append(tail)
                    while pending:
                        pending.pop(0)()
                    srcf = Wb["w_in"][l].rearrange("(c p) n -> p c n", p=128)[:, :, COL["bf"]:COL["bf"] + 6]
                    fw.dma(wf[:], srcf, reads=[R("wA", l, COL["bf"])], writes=[R("A", "wf")])
                    for sub in range(nsub):
                        t = half * nsub + sub
                        pb = (0, 1, 4, 5)[cnt["p"] % 4]
                        cnt["p"] += 1
                        for kc in range(KC):
                            fw.op("pe", lambda e, kc=kc, pb=pb, sub=sub: e.matmul(
                                P[pb][0:6, :], wf[:, kc, :], aT[:, kc, sub * 512:(sub + 1) * 512],
                                start=(kc == 0), stop=(kc == KC - 1)),
                                reads=[R("A", "wf"), R("A", "aT", sub, kc)], writes=[RP[pb]])
                        si = cnt["of"] % 2
                        cnt["of"] += 1
                        fw.op("act", lambda e, pb=pb, si=si: e.activation(s1[si][0:6, :], P[pb][0:6, :], AF.Sigmoid, bias=fbf[:, l:l + 1]),
                              reads=[RP[pb]], writes=[R("A", "s1", si)])
                        fw.op("act", lambda e, si=si: e.activation(of[si][0:6, :], s1[si][0:6, :], AF.Ln),
                              reads=[R("A", "s1", si)], writes=[R("A", "of", si)])
                        fw.dma(fx_f[:, t * 512:(t + 1) * 512], of[si][0:6, :], reads=[R("A", "of", si)], writes=[R("ff", t)])
                    for (nm, ncols, dst) in (("ai", 512, hg_v), ("bv", 768, fx_v), ("cv", 768, mb_v)):
                        for pc in range(ncols // 256):
                            wi = load_panel(COL[nm] + pc * 256)
                            for tb in range(TA // 128):
                                tbg = half * (TA // 128) + tb
                                sub = tb // 4
                                pb = (0, 1, 4, 5)[cnt["p"] % 4]
                                cnt["p"] += 1
                                for kc in range(KC):
                                    fw.op("pe", lambda e, kc=kc, wi=wi, pb=pb, tb=tb: e.matmul(
                                        P[pb][:, 0:256], aT[:, kc, tb * 128:(tb + 1) * 128], wp[wi][:, kc, :],
                                        start=(kc == 0), stop=(kc == KC - 1)),
                                        reads=[R("A", "wp", wi), R("A", "aT", sub, kc)], writes=[RP[pb]])
                                vi = cnt["vb"] % 2
                                cnt["vb"] += 1
                                eng = "act" if vi == 0 else "dve"
                                if eng == "act":
                                    fw.op("act", lambda e, pb=pb, vi=vi: e.copy(vb[vi][:], P[pb][:, 0:256]),
                                          reads=[RP[pb]], writes=[R("A", "vb", vi)])
                                else:
                                    fw.op("dve", lambda e, pb=pb, vi=vi: e.tensor_copy(vb[vi][:], P[pb][:, 0:256]),
                                          reads=[RP[pb]], writes=[R("A", "vb", vi)])
                                fw.dma(dst[tbg * 128:(tbg + 1) * 128, pc * 256:(pc + 1) * 256], vb[vi][:],
                                       reads=[R("A", "vb", vi)], writes=[R(nm, tbg, pc)])
            fw.barrier()

        def phase_hgrn(l):
          with ExitStack() as so_:
            IN = [dict(q=T(so_, f"hq{i}", [128, S], BF16), k=T(so_, f"hk{i}", [128, S], BF16), g=T(so_, f"hgt{i}", [128, S], BF16),
                       lf=T(so_, f"hlf{i}", [128, S]), v=T(so_, f"hv_{i}", [64, NCH, 128], BF16)) for i in range(2)]

            def hload(h):
                b_ = h % 2
                t_ = IN[b_]
                fw.dma(t_["q"][:], hg_q[h], reads=[R("aq", h, t) for t in range(NT)], writes=[R("H", "q", b_)])
                fw.dma(t_["k"][:], hg_k[h], reads=[R("hk", h, t) for t in range(NT)], writes=[R("H", "k", b_)])
                fw.dma(t_["g"][:], hg_g[h], reads=[R("ag", h, t) for t in range(NT)], writes=[R("H", "g", b_)])
                fw.dma(t_["lf"][:], hg_lf[h], reads=[R("hlf", h, t) for t in range(NT)], writes=[R("H", "lf", b_)])
                fw.dma(t_["v"][:], hg_v.rearrange("(c s) n -> s c n", s=64)[:, :, h * 128:(h + 1) * 128],
                       reads=[R("ai", tb, pc) for tb in range(NB) for pc in range(2)], writes=[R("H", "v", b_)])

            hload(0)
            for h in range(4):
                if h + 1 < 4:
                    hload(h + 1)
                hb_ = h % 2
                RQ, RK, RG, RV = R("H", "q", hb_), R("H", "k", hb_), R("H", "g", hb_), R("H", "v", hb_)
                with ExitStack() as st_:
                    q, k, g, v, lfin = IN[hb_]["q"], IN[hb_]["k"], IN[hb_]["g"], IN[hb_]["v"], IN[hb_]["lf"]
                    cs = [T(st_, f"cs{i}", [128, S]) for i in range(2)]
                    E = T(st_, "hE", [128, S])
                    qd = T(st_, "qd", [128, S], BF16); kd = T(st_, "kd", [128, S], BF16)
                    qe = T(st_, "qe", [128, S], BF16); klT = T(st_, "klT", [128, S], BF16)
                    oraw = cs[0]
                    ebl = T(st_, "ebl", [128, NCH])
                    stf = T(st_, "stf", [128, 128]); stb = [T(st_, f"stb{i}", [128, 128], BF16) for i in range(2)]
                    asb = [T(st_, f"asb{i}", [64, 64], BF16) for i in range(2)]
                    klsb = [T(st_, f"klsb{i}", [64, 128], BF16) for i in range(2)]
                    sq = T(st_, "hsq", [128, 512], BF16); rs = E[:, 0:512]; y = E[:, 512:1024]
                    yo = [T(st_, f"hyo{i}", [128, 512], BF16) for i in range(2)]
                    cur = 1
                    for d in (1, 2, 4, 8, 16, 32):
                        a3 = (lfin if d == 1 else cs[cur])[:].rearrange("p (c t) -> p c t", t=64)
                        b3 = cs[1 - cur][:].rearrange("p (c t) -> p c t", t=64)
                        rd = [R("H", "lf", hb_)] if d == 1 else [R("H", "cs", cur, "A"), R("H", "cs", cur, "C")]
                        fw.op("dve", lambda e, a3=a3, b3=b3, d=d: e.tensor_tensor(b3[:, :, d:64], a3[:, :, d:64], a3[:, :, 0:64 - d], ALU.add),
                              reads=rd, writes=[R("H", "cs", 1 - cur, "A")])
                        fw.op("pool", lambda e, a3=a3, b3=b3, d=d: e.tensor_copy(b3[:, :, 0:d], a3[:, :, 0:d]),
                              reads=rd, writes=[R("H", "cs", 1 - cur, "C")])
                        cur = 1 - cur
                    bb = cs[cur]
                    tmp = cs[1 - cur]
                    rb_ = [R("H", "cs", cur, "A"), R("H", "cs", cur, "C")]
                    rt_ = [R("H", "cs", 1 - cur, "A"), R("H", "cs", 1 - cur, "C")]
                    b3 = bb[:].rearrange("p (c t) -> p c t", t=64)
                    t3 = tmp[:].rearrange("p (c t) -> p c t", t=64)
                    fw.op("dve", lambda e: e.tensor_tensor(t3, b3, b3[:, :, 31:32].broadcast_to([128, NCH, 64]), ALU.subtract),
                          reads=rb_, writes=rt_)
                    fw.op("act", lambda e: e.activation(E[:], tmp[:], AF.Exp), reads=rt_, writes=[R("H", "E")])
                    fw.op("dve", lambda e: e.tensor_tensor(qd[:], q[:], E[:], ALU.mult), reads=[RQ, R("H", "E")], writes=[R("H", "qd")])
                    fw.op("act", lambda e: e.activation(E[:], tmp[:], AF.Exp, scale=-1.0), reads=rt_, writes=[R("H", "E")])
                    fw.op("dve", lambda e: e.tensor_tensor(kd[:], k[:], E[:], ALU.mult), reads=[RK, R("H", "E")], writes=[R("H", "kd")])
                    fw.op("act", lambda e: e.activation(E[:], bb[:], AF.Exp), reads=rb_, writes=[R("H", "E")])
                    fw.op("dve", lambda e: e.tensor_tensor(qe[:], q[:], E[:], ALU.mult), reads=[RQ, R("H", "E")], writes=[R("H", "qe")])
                    fw.op("dve", lambda e: e.tensor_tensor(t3, b3, b3[:, :, 63:64].broadcast_to([128, NCH, 64]), ALU.subtract),
                          reads=rb_, writes=rt_)
                    fw.op("act", lambda e: e.activation(E[:], tmp[:], AF.Exp, scale=-1.0), reads=rt_, writes=[R("H", "E")])
                    fw.op("dve", lambda e: e.tensor_tensor(klT[:], k[:], E[:], ALU.mult), reads=[RK, R("H", "E")], writes=[R("H", "klT")])
                    fw.op("act", lambda e: e.activation(ebl[:].rearrange("p (c o) -> p c o", o=1), b3[:, :, 63:64], AF.Exp), reads=rb_, writes=[R("H", "ebl")])
                    fw.op("dve", lambda e: e.memset(stf[:], 0.0), writes=[R("H", "stf")])
                    fw.op("dve", lambda e: e.memset(stb[0][:], 0.0), writes=[R("H", "stb", 0)])
                    for c in range(NCH):
                        csl = slice(c * 64, (c + 1) * 64)
                        pa = c % 2
                        fw.op("pe", lambda e, csl=csl, pa=pa: e.matmul(P[pa][0:64, 0:64], kd[:, csl], qd[:, csl], start=True, stop=True),
                              reads=[R("H", "kd"), R("H", "qd")], writes=[RP[pa]])
                        fw.op("dve", lambda e, pa=pa: e.tensor_tensor(asb[pa][:], P[pa][0:64, 0:64], hmask[:], ALU.mult),
                              reads=[RP[pa]], writes=[R("H", "asb", pa)])
                        fw.op("pe", lambda e, csl=csl, pa=pa: e.transpose(PB[0:64, pa * 128:(pa + 1) * 128], klT[:, csl], identb[:]),
                              reads=[R("H", "klT")], writes=[RPB[pa]])
                        fw.op("act", lambda e, pa=pa: e.copy(klsb[pa][:], PB[0:64, pa * 128:(pa + 1) * 128]),
                              reads=[RPB[pa]], writes=[R("H", "klsb", pa)])
                        fw.op("pe", lambda e, c=c, pa=pa: e.matmul(P[4 + pa][:, 0:128], klsb[pa][:], v[:, c, :], start=True, stop=True),
                              reads=[R("H", "klsb", pa), RV], writes=[RP[4 + pa]])
                        fw.op("dve", lambda e, c=c, pa=pa: e.scalar_tensor_tensor(stf[:], stf[:], ebl[:, c:c + 1], P[4 + pa][:, 0:128], ALU.mult, ALU.add),
                              reads=[RP[4 + pa], R("H", "ebl")], writes=[R("H", "stf")])
                        fw.op("act", lambda e, pa=pa: e.copy(stb[1 - pa][:], stf[:]),
                              reads=[R("H", "stf")], writes=[R("H", "stb", 1 - pa)])
                        fw.op("pe", lambda e, csl=csl, pa=pa: e.matmul(P[2 + pa][:, 0:64], stb[pa][:], qe[:, csl], start=True, stop=False),
                              reads=[R("H", "stb", pa), R("H", "qe")], writes=[RP[2 + pa]])
                        fw.op("pe", lambda e, c=c, pa=pa: e.matmul(P[2 + pa][:, 0:64], v[:, c, :], asb[pa][:], start=False, stop=True),
                              reads=[RV, R("H", "asb", pa)], writes=[RP[2 + pa]])
                        fw.op("act", lambda e, csl=csl, pa=pa: e.copy(oraw[:, csl], P[2 + pa][:, 0:64]),
                              reads=[RP[2 + pa]], writes=[R("H", "oraw", c // 8), R("H", "cs", 0, "A"), R("H", "cs", 0, "C")])
                    for t in range(NT):
                        tsl = slice(t * 512, (t + 1) * 512)
                        fw.op("act", lambda e, tsl=tsl: e.activation(sq[:], oraw[:, tsl], AF.Square), reads=[R("H", "oraw", t)], writes=[R("H", "sq")])
                        fw.op("pe", lambda e: e.matmul(P[6][:], onesH[:], sq[:], start=True, stop=True), reads=[R("H", "sq")], writes=[RP[6]])
                        fw.op("act", lambda e: e.activation(rs[:], P[6][:], AF.Ln, bias=EPS), reads=[RP[6]], writes=[R("H", "rs"), R("H", "E")])
                        fw.op("act", lambda e: e.activation(rs[:], rs[:], AF.Exp, scale=-0.5), reads=[R("H", "rs")], writes=[R("H", "rs")])
                        fw.op("dve", lambda e, tsl=tsl: e.scalar_tensor_tensor(y[:], oraw[:, tsl], hv[:, 0, l:l + 1], rs[:], ALU.mult, ALU.mult),
                              reads=[R("H", "oraw", t), R("H", "rs")], writes=[R("H", "y")])
                        yi = t % 2
                        fw.op("dve", lambda e, tsl=tsl, yi=yi: e.tensor_tensor(yo[yi][:], y[:], g[:, tsl], ALU.mult),
                              reads=[R("H", "y"), RG], writes=[R("H", "yo", yi)])
                        fw.dma(mixT[h * 128:(h + 1) * 128, tsl], yo[yi][:], reads=[R("H", "yo", yi)], writes=[R("mix", h, t)])
                fw.barrier()

        def phase_attn(l):
            with ExitStack() as sl_:
                nfk = T(sl_, "nfk", [128, NB, 6])
                qterm = T(sl_, "qterm", [128, 6, 512]); kcol = T(sl_, "kcol", [128, 6, 40])
                pastm = T(sl_, "pastm", [128, 32, 16]); ownm = T(sl_, "ownm", [128, 32, 16])
                cmaskb = T(sl_, "cmaskb", [128, 4, 512], BF16)
                AC = dict(cmaskb=cmaskb, qterm=qterm, kcol=kcol, indb=indb, pastm=pastm, ownm=ownm)
                with ExitStack() as sf_:
                    cmask = T(sf_, "cmask", [128, 4, 512])
                    fc = [T(sf_, f"fc{i}", [6, S]) for i in range(2)]
                    for dst, src in ((cmask, cmask_d), (qterm, qterm_d), (kcol, kcol_d), (pastm, pastm_d), (ownm, ownm_d)):
                        fw.dma(dst[:], src, writes=[R("X", "c", len(fw.res))])
                    fw.barrier()
                    fw.op("dve", lambda e: e.tensor_scalar(cmaskb[:], cmask[:], -SELB / NEGM, 0.0, ALU.mult, ALU.add), writes=[R("X", "cmaskb")])
                    fw.dma(fc[0][:], fx_f, reads=[R("ff", t) for t in range(NT)], writes=[R("X", "fc", 0, "A"), R("X", "fc", 0, "C")])
                    cur = 0
                    d = 1
                    while d < S:
                        rd = [R("X", "fc", cur, "A"), R("X", "fc", cur, "C")]
                        fw.op("dve", lambda e, cur=cur, d=d: e.tensor_tensor(fc[1 - cur][:, d:S], fc[cur][:, d:S], fc[cur][:, 0:S - d], ALU.add),
                              reads=rd, writes=[R("X", "fc", 1 - cur, "A")])
                        fw.op("pool", lambda e, cur=cur, d=d: e.tensor_copy(fc[1 - cur][:, 0:d], fc[cur][:, 0:d]),
                              reads=rd, writes=[R("X", "fc", 1 - cur, "C")])
                        cur = 1 - cur
                        d *= 2
                    rfc = [R("X", "fc", cur, "A"), R("X", "fc", cur, "C")]
                    fw.dma(fx_fc, fc[cur][:], reads=rfc, writes=[R("ffc")])
                    for j in range(NB):
                        fw.op("pe", lambda e, j=j: e.transpose(P[6][:, 0:6], fc[cur][:, j * 128:(j + 1) * 128], identf[0:6, 0:6]),
                              reads=rfc, writes=[RP[6]])
                        fw.op("dve", lambda e, j=j: e.tensor_scalar(nfk[:, j, :], P[6][:, 0:6], -1.0, 0.0, ALU.mult, ALU.add),
                              reads=[RP[6]], writes=[R("X", "nfk")])
                    fw.barrier()
                AC["q"] = [T(sl_, f"xq{i}", [128, S], BF16) for i in range(2)]
                AC["k"] = [T(sl_, f"xk{i}", [128, S], BF16) for i in range(2)]
                AC["v"] = [T(sl_, f"xv{i}", [128, NB, 128], BF16) for i in range(2)]
                AC["fqb"] = [T(sl_, f"fqb{i}", [128, S]) for i in range(2)]
                AC["x"] = [T(sl_, f"xx{i}", [128, 512]) for i in range(6)]
                AC["pt"] = [T(sl_, f"xp{i}", [128, 512], BF16) for i in range(6)]
                AC["rden"] = [T(sl_, f"rden{i}", [128, 512]) for i in range(2)]
                AC["ot"] = [T(sl_, f"xo{i}", [128, 512], BF16) for i in range(2)]
                AC["kb32"] = T(sl_, "kb32", [128, 16]); AC["kbar"] = T(sl_, "kbar", [128, 16], BF16)
                AC["gm"] = T(sl_, "gm", [128, 32, 16]); AC["m8"] = T(sl_, "m8", [128, 32, 8])
                AC["sel"] = T(sl_, "sel", [128, 32, 16]); AC["selb"] = T(sl_, "selb", [128, 32, 32], BF16)
                AC["selF"] = T(sl_, "selF", [128, 512])
                AC["selT"] = [T(sl_, f"selT{i}", [128, S], BF16) for i in range(2)]
                XS = os.environ.get("XSUB", "FM")
                hcnt = [0]
                if l + 1 < L and "w" in os.environ.get("PHASES", "wAHXC"):
                    fw.op("dve", lambda e: e.memset(gate_t[:], 0.0), writes=[R("gate", l)])
                    cast_weights(l + 1, gate=R("gate", l))
                hlist = [(kind, h) for kind in ("fox", "moba") if {"fox": "F", "moba": "M"}[kind] in XS for h in range(6)]
                if hlist:
                    attn_load(hlist[0][0], hlist[0][1], AC, 0)
                for hi_, (kind, h) in enumerate(hlist):
                    if hi_ + 1 < len(hlist):
                        attn_load(hlist[hi_ + 1][0], hlist[hi_ + 1][1], AC, hi_ + 1)
                    attn_head(l, kind, h, nfk, AC, hi_)
            fw.barrier()

        def attn_load(kind, h, AC, hc):
            fox = kind == "fox"
            qs, ks, vs = (fx_q, fx_k, fx_v) if fox else (mb_q, mb_k, mb_v)
            qn, kn, vn = ("bq", "bk", "bv") if fox else ("cq", "ck", "cv")
            hb = hc % 2
            q, k, v, fqb = AC["q"][hb], AC["k"][hb], AC["v"][hb], AC["fqb"][hb]
            Rq, Rk, Rv, Rf = R("X", "q", hb), R("X", "k", hb), R("X", "v", hb), R("X", "fqb", hb)
            fw.dma(q[:], qs[h], reads=[R(qn, h, t) for t in range(NT)], writes=[Rq])
            fw.dma(k[:], ks[h], reads=[R(kn, h, t) for t in range(NT)], writes=[Rk])
            fw.dma(v[:], vs.rearrange("(j p) n -> p j n", p=128)[:, :, h * 128:(h + 1) * 128],
                   reads=[R(vn, tb, pc) for tb in range(NB) for pc in range(3)], writes=[Rv])
            if fox:
                fw.dma(fqb[:], fx_fc[h:h + 1, :].broadcast_to([128, S]), reads=[R("ffc")], writes=[Rf])

        def attn_head(l, kind, h, nfk, AC, hc):
            cmaskb, qterm, kcol, indb, pastm, ownm = (AC[k_] for k_ in ("cmaskb", "qterm", "kcol", "indb", "pastm", "ownm"))
            fox = kind == "fox"
            qs, ks, vs = (fx_q, fx_k, fx_v) if fox else (mb_q, mb_k, mb_v)
            qn, kn, vn = ("bq", "bk", "bv") if fox else ("cq", "ck", "cv")
            mrow = (4 + h) * 128 if fox else (10 + h) * 128
            hb = hc % 2
            q, k, v, fqb = AC["q"][hb], AC["k"][hb], AC["v"][hb], AC["fqb"][hb]
            x, pt, rden, ot = AC["x"], AC["pt"], AC["rden"], AC["ot"]
            Rq, Rk, Rv, Rf = R("X", "q", hb), R("X", "k", hb), R("X", "v", hb), R("X", "fqb", hb)
            if not fox:
                kb32, kbar, gm, m8, sel, selb, selF = (AC[k_] for k_ in ("kb32", "kbar", "gm", "m8", "sel", "selb", "selF"))
                selT = AC["selT"][hb]
                RsT = R("M", "selT", hb)
                nblk = S // 256
                fw.op("dve", lambda e: e.memset(kb32[:], 0.0), writes=[R("M", "kb32")])
                fw.op("dve", lambda e: e.memset(selb[:], 0.0), writes=[R("M", "selb")])
                fw.op("dve", lambda e: e.tensor_reduce(kb32[:, 0:nblk], k[:].rearrange("p (n t) -> p n t", t=256), AX.X, ALU.add),
                      reads=[Rk], writes=[R("M", "kb32")])
                fw.op("act", lambda e: e.mul(kbar[:], kb32[:], 1.0 / 256), reads=[R("M", "kb32")], writes=[R("M", "kbar")])
                for qb in range(NB):
                    fw.op("pe", lambda e, qb=qb: e.matmul(P[6][:, qb * 16:(qb + 1) * 16], q[:, qb * 128:(qb + 1) * 128], kbar[:], start=True, stop=True),
                          reads=[Rq, R("M", "kbar")], writes=[RP[6]])
                g2 = gm[:].rearrange("p a b -> p (a b)")
                fw.op("dve", lambda e: e.tensor_tensor(g2[:, 0:NB * 16], P[6][:, 0:NB * 16], pastm[:].rearrange("p a b -> p (a b)")[:, 0:NB * 16], ALU.add),
                      reads=[RP[6]], writes=[R("M", "gm")])
                for qb in range(NB):
                    fw.op("dve", lambda e, qb=qb: e.max(m8[:, qb, :], gm[:, qb, :]), reads=[R("M", "gm")], writes=[R("M", "m8")])
                fw.op("dve", lambda e: e.tensor_tensor(sel[:, 0:NB, :], gm[:, 0:NB, :], m8[:, 0:NB, 2:3].broadcast_to([128, NB, 16]), ALU.is_ge),
                      reads=[R("M", "gm"), R("M", "m8")], writes=[R("M", "sel")])
                fw.op("dve", lambda e: e.tensor_scalar(sel[:, 0:NB, :], sel[:, 0:NB, :], SELB, -SELB, ALU.mult, ALU.add),
                      reads=[R("M", "sel")], writes=[R("M", "sel")])
                fw.op("dve", lambda e: e.tensor_tensor(selb[:, 0:NB, 0:16], sel[:, 0:NB, :], ownm[:, 0:NB, :], ALU.max),
                      reads=[R("M", "sel")], writes=[R("M", "selb")])
                for g4 in range(NB // 4):
                    for pa in range(4):
                        qb = g4 * 4 + pa
                        fw.op("pe", lambda e, qb=qb, pa=pa: e.matmul(P[6][0:32, pa * 128:(pa + 1) * 128], selb[:, qb, :], identb[:], start=True, stop=True),
                              reads=[R("M", "selb"), R("M", "selF")], writes=[RP[6]])
                    fw.op("dve", lambda e: e.tensor_copy(selF[:], P[6][:]), reads=[RP[6]], writes=[R("M", "selF")])
                    fw.op("dve", lambda e, g4=g4: e.tensor_copy(selT[0:32, g4 * 512:(g4 + 1) * 512], selF[0:32, :]),
                          reads=[R("M", "selF")], writes=[RsT])
            pairs = [(j, kb) for j in range(NT) for kb in range(4 * j + 4)]
            npair = len(pairs)
            LA = 3
            NSB = 4
            NXB = 6

            def isdiag(i):
                j, kb = pairs[i]
                return kb >= 4 * j

            def qk_main(i):
                j, kb = pairs[i]
                sb = i % NSB
                qsl = slice(j * 512, (j + 1) * 512)
                fw.op("pe", lambda e: e.matmul(P[sb][:], k[:, kb * 128:(kb + 1) * 128], q[:, qsl], start=True, stop=(fox and not isdiag(i))),
                      reads=[Rk, Rq], writes=[RP[sb]])

            def qk_sel(i):
                j, kb = pairs[i]
                sb = i % NSB
                qsl = slice(j * 512, (j + 1) * 512)
                fw.op("pe", lambda e: e.matmul(P[sb][:], indb[:, kb // 2, :], selT[0:32, qsl], start=False, stop=(not isdiag(i))),
                      reads=[RsT], writes=[RP[sb]])

            def qk_mask(i):
                j, kb = pairs[i]
                sb = i % NSB
                r = kb - 4 * j
                fw.op("pe", lambda e: e.matmul(P[sb][:], identb[:], cmaskb[:, r, :], start=False, stop=True), writes=[RP[sb]])

            def emit_soft(i):
                j, kb = pairs[i]
                sb = i % NSB
                xb = i % NXB
                qsl = slice(j * 512, (j + 1) * 512)
                if fox:
                    fw.op("dve", lambda e: e.tensor_tensor(x[xb][:], P[sb][:], fqb[:, qsl], ALU.add),
                          reads=[RP[sb], Rf], writes=[R("X", "x", xb)])
                    bias = nfk[:, kb, h:h + 1]
                else:
                    fw.op("dve", lambda e: e.tensor_tensor(x[xb][:], P[sb][:], qterm[:, h, :], ALU.add),
                          reads=[RP[sb]], writes=[R("X", "x", xb)])
                    delta = 4 * j - kb
                    bias = kcol[:, h, delta + 3:delta + 4]
                fw.op("act", lambda e: e.activation(pt[xb][:], x[xb][:], AF.Exp, bias=bias),
                      reads=[R("X", "x", xb), R("X", "nfk")], writes=[R("X", "pt", xb)])

            def pv(i):
                j, kb = pairs[i]
                xb = i % NXB
                nkb = 4 * j + 4
                po = 4 + (j % 2)
                fw.op("pe", lambda e: e.matmul(P[po][:], v[:, kb, :], pt[xb][:], start=(kb == 0), stop=(kb == nkb - 1)),
                      reads=[Rv, R("X", "pt", xb)], writes=[RP[po]])

            def den(i, it):
                j, kb = pairs[i]
                xb = i % NXB
                nkb = 4 * j + 4
                po = 4 + (j % 2)
                pd = 6
                fw.op("pe", lambda e: e.matmul(P[pd][:], ones1[:], pt[xb][:], start=(kb == 0), stop=(kb == nkb - 1)),
                      reads=[R("X", "pt", xb)], writes=[RP[pd]])
                if kb == nkb - 1:
                    oi = j % 2
                    qsl = slice(j * 512, (j + 1) * 512)
                    fw.op("act", lambda e: e.activation(rden[oi][:], P[pd][:], AF.Ln), reads=[RP[pd]], writes=[R("X", "rden", oi)])
                    fw.op("act", lambda e: e.activation(rden[oi][:], rden[oi][:], AF.Exp, scale=-1.0),
                          reads=[R("X", "rden", oi)], writes=[R("X", "rden", oi)])

                    def fin():
                        fw.op("dve", lambda e: e.tensor_tensor(ot[oi][:], P[po][:], rden[oi][:], ALU.mult),
                              reads=[RP[po], R("X", "rden", oi)], writes=[R("X", "ot", oi)])
                        fw.dma(mixT[mrow:mrow + 128, qsl], ot[oi][:], reads=[R("X", "ot", oi)], writes=[R("mix", mrow // 128, j)])
                    fins.append((it + 3, fin))

            fins = []
            for i in range(npair + LA):
                ii = i - LA
                if ii >= 0:
                    emit_soft(ii)
                while fins and fins[0][0] <= i:
                    fins.pop(0)[1]()
                if i < npair:
                    qk_main(i)
                if ii >= 0:
                    pv(ii)
                if i < npair and not fox:
                    qk_sel(i)
                if ii >= 0:
                    den(ii, i)
                if i < npair and isdiag(i):
                    qk_mask(i)
            while fins:
                fins.pop(0)[1]()

        def phase_C(l, last):
            hsrc = xT if l == 0 else hbuf
            hdst = outT if last else hbuf
            hs3 = hsrc.rearrange("(c p) s -> p c s", p=128)
            hd3 = hdst.rearrange("(c p) s -> p c s", p=128)
            mx3 = mixT.rearrange("(c p) s -> p c s", p=128)
            with ExitStack() as st_:
                ht = T(st_, "ht", [128, KC, 512])
                ct = T(st_, "ct", [128, KC, 512], BF16)
                mt = ct
                st = dict(sq0=T(st_, "csq0", [128, 512], BF16), sq1=T(st_, "csq1", [128, 512], BF16),
                          rs=T(st_, "crs", [128, 512]), rstd=T(st_, "crstd", [128, 512]))
                hid = T(st_, "hid", [128, FC, 512], BF16)
                wp = [T(st_, f"cwp{i}", [128, KC, 256], BF16) for i in range(3)]
                wd = [T(st_, f"cwd{i}", [128, FC, 128], BF16) for i in range(2)]
                wq = T(st_, "cwq", [128, 2, D], BF16)
                hg = [T(st_, f"hg{i}", [128, 514]) for i in range(2)]
                halo = T(st_, "halo", [128, FC, 2])
                yy = [T(st_, f"yy{i}", [128, 512]) for i in range(2)]
                gl = [T(st_, f"gl{i}", [128, 512]) for i in range(2)]
                pf = T(st_, "pf", [128, 2, 512]); pb16 = T(st_, "pb16", [128, 2, 512], BF16)
                sg = [T(st_, f"sg{i}", [128, 512]) for i in range(2)]
                cnt = dict(wp=0, wd=0, p=0, e=0)
                fw.op("dve", lambda e: e.memset(halo[:], 0.0), writes=[R("C", "halo")])
                fw.dma(wq[:], Wb["w_pp"][l].rearrange("(c p) n -> p c n", p=128), reads=wres("w_pp", l), writes=[R("C", "wq")])

                def load_panel(wname, col0):
                    i = cnt["wp"] % 3
                    cnt["wp"] += 1
                    src = Wb[wname][l].rearrange("(c p) n -> p c n", p=128)[:, :, col0:col0 + 256]
                    fw.dma(wp[i][:], src, reads=wres(wname, l), writes=[R("C", "wp", i)])
                    return i

                def mm16(pb, wi, g, rhs, rres):
                    for kc in range(KC):
                        fw.op("pe", lambda e, kc=kc: e.matmul(P[pb][:], wp[wi][:, kc, g * 128:(g + 1) * 128], rhs[:, kc, :],
                                                              start=(kc == 0), stop=(kc == KC - 1)),
                              reads=[R("C", "wp", wi), R("C", "ct", kc)], writes=[RP[pb]])

                for t in range(NT):
                    tsl = slice(t * 512, (t + 1) * 512)
                    fw.dma(ht[:], hs3[:, :, tsl], reads=[R("h", t)], writes=[R("C", "ht")])
                    fw.dma(mt[:], mx3[:, :, tsl], reads=[R("mix", c, t) for c in range(KC)], writes=[R("C", "ct", c) for c in range(KC)])
                    fw.dma(pf[:], pT[l].rearrange("(c p) s -> p c s", p=128)[:, :, tsl], writes=[R("C", "pf")])
                    fw.op("pool", lambda e: e.tensor_copy(pb16[:], pf[:]), reads=[R("C", "pf")], writes=[R("C", "pb16")])
                    for pc in range(D // 256):
                        wi = load_panel("w_out", pc * 256)
                        for g in range(2):
                            c = pc * 2 + g
                            pb = cnt["p"] % 2
                            cnt["p"] += 1
                            mm16(pb, wi, g, mt, R("C", "ct"))
                            fw.op("dve", lambda e, c=c, pb=pb: e.tensor_tensor(ht[:, c, :], ht[:, c, :], P[pb][:], ALU.add),
                                  reads=[RP[pb]], writes=[R("C", "ht")])
                    rmsnorm_tile(st, ht, gF, l, lambda c: ct[:, c, :], R("C", "ht"), (lambda c: R("C", "ct", c)), "Cn")
                    for pc in range(DFF // 256):
                        wg = load_panel("w_gate", pc * 256)
                        wu = load_panel("w_up", pc * 256)
                        for g in range(2):
                            f = pc * 2 + g
                            ei = cnt["e"] % 2
                            cnt["e"] += 1
                            pg_, pu_ = (0, 1) if ei == 0 else (4, 5)
                            mm16(pg_, wg, g, ct, R("C", "ct"))
                            fw.op("act", lambda e, ei=ei, pg_=pg_: e.copy(hg[ei][:, 2:514], P[pg_][:]), reads=[RP[pg_]], writes=[R("C", "hg", ei)])
                            mm16(pu_, wu, g, ct, R("C", "ct"))
                            fw.op("pool", lambda e, ei=ei, f=f: e.tensor_copy(hg[ei][:, 0:2], halo[:, f, :]),
                                  reads=[R("C", "halo")], writes=[R("C", "hg", ei)])
                            fw.op("act", lambda e, ei=ei, f=f: e.activation(yy[ei][:], hg[ei][:, 2:514], AF.Identity,
                                                                            bias=convb[:, l, f:f + 1], scale=convw[:, l, 2, f:f + 1]),
                                  reads=[R("C", "hg", ei)], writes=[R("C", "yy", ei)])
                            fw.op("dve", lambda e, ei=ei, f=f: e.scalar_tensor_tensor(yy[ei][:], hg[ei][:, 1:513], convw[:, l, 1, f:f + 1], yy[ei][:], ALU.mult, ALU.add),
                                  reads=[R("C", "hg", ei), R("C", "yy", ei)], writes=[R("C", "yy", ei)])
                            fw.op("dve", lambda e, ei=ei, f=f: e.scalar_tensor_tensor(yy[ei][:], hg[ei][:, 0:512], convw[:, l, 0, f:f + 1], yy[ei][:], ALU.mult, ALU.add),
                                  reads=[R("C", "hg", ei), R("C", "yy", ei)], writes=[R("C", "yy", ei)])
                            fw.op("pool", lambda e, ei=ei, f=f: e.tensor_copy(halo[:, f, :], hg[ei][:, 512:514]),
                                  reads=[R("C", "hg", ei)], writes=[R("C", "halo")])
                            fw.op("act", lambda e, ei=ei: e.activation(gl[ei][:], yy[ei][:], AF.Gelu_apprx_tanh),
                                  reads=[R("C", "yy", ei)], writes=[R("C", "gl", ei)])
                            fw.op("dve", lambda e, ei=ei, f=f, pu_=pu_: e.tensor_tensor(hid[:, f, :], gl[ei][:], P[pu_][:], ALU.mult),
                                  reads=[R("C", "gl", ei), RP[pu_]], writes=[R("C", "hid")])
                    for c in range(KC):
                        di = cnt["wd"] % 2
                        cnt["wd"] += 1
                        fw.dma(wd[di][:], Wb["w_down"][l].rearrange("(f p) n -> p f n", p=128)[:, :, c * 128:(c + 1) * 128],
                               reads=wres("w_down", l), writes=[R("C", "wd", di)])
                        pb = 2 + c % 2
                        for f in range(FC):
                            fw.op("pe", lambda e, f=f, di=di, pb=pb: e.matmul(P[pb][:], wd[di][:, f, :], hid[:, f, :], start=(f == 0), stop=(f == FC - 1)),
                                  reads=[R("C", "wd", di), R("C", "hid")], writes=[RP[pb]])
                        fw.op("dve", lambda e, c=c, pb=pb: e.tensor_tensor(ht[:, c, :], ht[:, c, :], P[pb][:], ALU.add),
                              reads=[RP[pb]], writes=[R("C", "ht")])
                    rmsnorm_tile(st, ht, gP, l, lambda c: ct[:, c, :], R("C", "ht"), (lambda c: R("C", "ct", c)), "Cn")
                    for pc in range(D // 256):
                        wi = load_panel("w_pg", pc * 256)
                        for g in range(2):
                            c = pc * 2 + g
                            pb = cnt["p"] % 2
                            cnt["p"] += 1
                            si = c % 2
                            mm16(pb, wi, g, ct, R("C", "ct"))
                            fw.op("act", lambda e, pb=pb, si=si: e.activation(sg[si][:], P[pb][:], AF.Sigmoid), reads=[RP[pb]], writes=[R("C", "sg", si)])
                            p2 = 4 + c % 2
                            for kc in range(2):
                                fw.op("pe", lambda e, kc=kc, c=c, p2=p2: e.matmul(P[p2][:], wq[:, kc, c * 128:(c + 1) * 128], pb16[:, kc, :], start=(kc == 0), stop=(kc == 1)),
                                      reads=[R("C", "wq"), R("C", "pb16")], writes=[RP[p2]])
                            fw.op("dve", lambda e, si=si, p2=p2: e.tensor_tensor(sg[si][:], sg[si][:], P[p2][:], ALU.mult),
                                  reads=[R("C", "sg", si), RP[p2]], writes=[R("C", "sg", si)])
                            fw.op("dve", lambda e, c=c, si=si: e.tensor_tensor(ht[:, c, :], ht[:, c, :], sg[si][:], ALU.add),
                                  reads=[R("C", "sg", si)], writes=[R("C", "ht")])
                    fw.dma(hd3[:, :, tsl], ht[:], reads=[R("C", "ht")], writes=[R("h", t)])
            fw.barrier()

        PH = os.environ.get("PHASES", "wAHXC")
        if "w" in PH:
            cast_weights(0)
        for l in range(L):
            if "A" in PH:
                phase_A(l)
            if "H" in PH:
                phase_hgrn(l)
            if "X" in PH:
                phase_attn(l)
            if "C" in PH:
                phase_C(l, l == L - 1)
        fw.barrier(all_dma=True)
        print("instructions:", fw.ninst, {k: v for k, v in fw.ccount.items()})
    return nc


def host_consts():
    slopes = np.exp2(-8.0 * np.arange(1, 7, dtype=np.float32) / 6).astype(np.float32)
    p = np.arange(128)
    q = np.arange(512)
    cmask = np.zeros((128, 4, 512), np.float32)
    for r in range(4):
        cmask[:, r, :] = np.where((r * 128 + p)[:, None] <= q[None, :], 0.0, NEGM)
    hmask = (np.arange(64)[:, None] <= np.arange(64)[None, :]).astype(np.float32)
    qterm = np.broadcast_to((-slopes[:, None] * q[None, :].astype(np.float32))[None], (128, 6, 512)).astype(np.float32).copy()
    delta = np.arange(-3, 37).astype(np.float32)
    kcol = (slopes[None, :, None] * (p[:, None, None].astype(np.float32) - 128.0 * delta[None, None, :])).astype(np.float32)
    ind = np.zeros((32, 16, 128), np.float32)
    for n in range(16):
        ind[n, n, :] = 1.0
    own = (np.arange(32) // 2)[:, None]
    n = np.arange(16)[None, :]
    pastm = np.broadcast_to(np.where(n < own, 0.0, NEGM)[None], (128, 32, 16)).astype(np.float32).copy()
    ownm = np.broadcast_to(np.where(n < own, -SELB, 0.0)[None], (128, 32, 16)).astype(np.float32).copy()
    return dict(ident=np.eye(128, dtype=np.float32), cmask=cmask, hmask=hmask, qterm=qterm, kcol=kcol, ind=ind,
                pastm=pastm, ownm=ownm)


def pm(v, L):
    v = np.asarray(v, np.float32)
    return np.ascontiguousarray(v.reshape(L, -1, 128).transpose(2, 0, 1))


def make_in_maps(inp, S, L, nb):
    f32 = lambda a: np.ascontiguousarray(np.asarray(a, np.float32))
    shared = dict(
        w_in=f32(inp["w_in"][:L]), w_out=f32(inp["w_out"][:L]), w_gate=f32(inp["w_gate"][:L]), w_up=f32(inp["w_up"][:L]),
        w_down=f32(inp["w_down"][:L]), w_pg=f32(inp["w_ple_gate"][:L]), w_pp=f32(inp["w_ple_proj"][:L]),
        gA=pm(inp["attn_norm"][:L], L), gF=pm(inp["ffn_norm"][:L], L), gP=pm(inp["ple_norm"][:L], L),
        convw=np.ascontiguousarray(np.asarray(inp["conv_w"][:L], np.float32).reshape(L, 3, FC, 128).transpose(3, 0, 1, 2)),
        convb=pm(inp["conv_b"][:L], L), lbl=pm(inp["lb_logits"][:L], L),
        hv=np.ascontiguousarray(np.stack([np.asarray(inp[k][:L], np.float32).T for k in
                                          ("hgrn_onorm", "fox_qnorm", "fox_knorm", "moba_qnorm", "moba_knorm")], axis=1)),
        fbf=np.ascontiguousarray(np.asarray(inp["fox_bf"][:L], np.float32).T),
        **host_consts())
    maps = []
    for b in range(nb):
        m = dict(shared)
        m["xT"] = np.ascontiguousarray(np.asarray(inp["x"][b, :S], np.float32).T)
        m["pT"] = np.ascontiguousarray(np.asarray(inp["p"][:L, b, :S], np.float32).transpose(0, 2, 1))
        maps.append(m)
    return maps


_NC_CACHE = {}


def kernel(**inputs):
    S, L, B = 4096, 4, 4
    if (S, L) not in _NC_CACHE:
        _NC_CACHE[(S, L)] = build(S, L)
    nc = _NC_CACHE[(S, L)]
    maps = make_in_maps(inputs, S, L, B)
    res = run_bass_kernel_spmd(nc, maps, core_ids=list(range(B)))
    out = np.stack([np.asarray(r["outT"], np.float32).T for r in res.results], axis=0)
    return np.ascontiguousarray(out)
```
